# Optimizing a Trainium2 kernel written in Bass

```python
import math
import jax
import jax.numpy as jnp
from jax import lax
import numpy as np

D_MODEL = 4096
BATCH = 2
SEQ = 8192
DEPTH = 2

HEAD_DIM = 128
ROT_DIM = HEAD_DIM // 4
ROPE_THETA = 500000.0

A_HEADS = D_MODEL // (4 * HEAD_DIM)
A_WIDTH = A_HEADS * 2 * HEAD_DIM
B_HEADS = D_MODEL // (2 * HEAD_DIM)
B_WIDTH = B_HEADS * HEAD_DIM
MOBA_BLOCK = 256
MOBA_TOPK = 3
MOBA_Q_CHUNK = 16
Q_BLOCK = 128

C_HEADS = D_MODEL // HEAD_DIM
C_KV_HEADS = C_HEADS // 4
C_Q_WIDTH = C_HEADS * HEAD_DIM
C_KV_WIDTH = C_KV_HEADS * HEAD_DIM
WINDOW = 128

N_EXPERTS = 16
N_GROUPS = 4
EXPERTS_PER_GROUP = N_EXPERTS // N_GROUPS
TOPK_GROUPS = 1
TOPK_EXPERTS = 2
D_FF_EXPERT = D_MODEL // 4

DN_ALPHA = float((2 * DEPTH) ** 0.25)
DN_BETA = float((8 * DEPTH) ** -0.25)
LN_EPS = 1e-5
RMS_EPS = 1e-5

kernel_name = "hybrid_diff_moba_swa_moe_deepnorm"


def layer_norm(x, g, b):
    xf = x.astype(jnp.float32)
    mu = jnp.mean(xf, axis=-1, keepdims=True)
    var = jnp.mean(jnp.square(xf - mu), axis=-1, keepdims=True)
    y = (xf - mu) * lax.rsqrt(var + LN_EPS) * g.astype(jnp.float32) + b.astype(jnp.float32)
    return y.astype(x.dtype)


def adaln_modulation(c, w_ada, b_ada):
    m = jax.nn.silu(c) @ w_ada + b_ada
    return jnp.split(m[:, None, :], 6, axis=-1)


def rope_tables(positions):
    inv_freq = ROPE_THETA ** (-jnp.arange(0, ROT_DIM, 2, dtype=jnp.float32) / ROT_DIM)
    ang = positions.astype(jnp.float32)[..., None] * inv_freq
    return jnp.cos(ang), jnp.sin(ang)


def apply_partial_rope(x, cos, sin):
    half = ROT_DIM // 2
    xr = x[..., :ROT_DIM].astype(jnp.float32)
    x1, x2 = xr[..., :half], xr[..., half:]
    cs = cos[:, :, None, :]
    sn = sin[:, :, None, :]
    rot = jnp.concatenate([x1 * cs - x2 * sn, x2 * cs + x1 * sn], axis=-1).astype(x.dtype)
    return jnp.concatenate([rot, x[..., ROT_DIM:]], axis=-1)


def diff_attention(q, k, v, lam, subln_g, lam_init):
    Bn, S, H = q.shape[0], q.shape[1], q.shape[2]
    scale = HEAD_DIM ** -0.5
    nq = S // Q_BLOCK
    qb = q.reshape(Bn, nq, Q_BLOCK, H, 2, HEAD_DIM).transpose(1, 0, 2, 3, 4, 5)
    kpos = jnp.arange(S)

    def block(args):
        qi, i = args
        s = jnp.einsum('bqhmd,bkhmd->bhmqk', qi, k).astype(jnp.float32) * scale
        qpos = i * Q_BLOCK + jnp.arange(Q_BLOCK)
        s = jnp.where(kpos[None, :] <= qpos[:, None], s, -jnp.inf)
        p = jax.nn.softmax(s, axis=-1)
        a = p[:, :, 0] - lam * p[:, :, 1]
        return jnp.einsum('bhqk,bkhe->bqhe', a.astype(v.dtype), v)

    o = lax.map(block, (qb, jnp.arange(nq)))
    o = o.transpose(1, 0, 2, 3, 4).reshape(Bn, S, H, 2 * HEAD_DIM)
    of = o.astype(jnp.float32)
    of = of * lax.rsqrt(jnp.mean(of * of, axis=-1, keepdims=True) + RMS_EPS) * subln_g.astype(jnp.float32)
    return (of * (1.0 - lam_init)).astype(q.dtype)


def moba_attention(q, k, v):
    Bn, S, H, D = q.shape
    scale = HEAD_DIM ** -0.5
    nb = -(-S // MOBA_BLOCK)
    pad = nb * MOBA_BLOCK - S
    kp = jnp.pad(k, ((0, 0), (0, pad), (0, 0), (0, 0)))
    vp = jnp.pad(v, ((0, 0), (0, pad), (0, 0), (0, 0)))
    kblk = kp.reshape(Bn, nb, MOBA_BLOCK, H, D).transpose(0, 3, 1, 2, 4)
    vblk = vp.reshape(Bn, nb, MOBA_BLOCK, H, D).transpose(0, 3, 1, 2, 4)
    kmean = jnp.mean(kblk.astype(jnp.float32), axis=3)
    ksel = min(MOBA_TOPK, max(nb - 1, 1))
    nc = S // MOBA_Q_CHUNK
    qc = q.reshape(Bn, nc, MOBA_Q_CHUNK, H, D).transpose(1, 0, 3, 2, 4)
    bi = jnp.arange(Bn)[:, None, None, None]
    hi = jnp.arange(H)[None, :, None, None]

    def chunk(args):
        qi, ci = args
        start = ci * MOBA_Q_CHUNK
        own = start // MOBA_BLOCK
        gate = jnp.einsum('bhqd,bhnd->bhqn', qi.astype(jnp.float32), kmean)
        gate = jnp.where(jnp.arange(nb) < own, gate, -jnp.inf)
        _, idx = lax.top_k(gate, ksel)
        sel_valid = jnp.arange(ksel) < own
        k_sel = kblk[bi, hi, idx]
        v_sel = vblk[bi, hi, idx]
        k_own = lax.dynamic_index_in_dim(kblk, own, axis=2, keepdims=False)
        v_own = lax.dynamic_index_in_dim(vblk, own, axis=2, keepdims=False)
        s_sel = jnp.einsum('bhqd,bhqnkd->bhqnk', qi, k_sel).astype(jnp.float32) * scale
        s_sel = jnp.where(sel_valid[:, None], s_sel, -jnp.inf)
        s_own = jnp.einsum('bhqd,bhkd->bhqk', qi, k_own).astype(jnp.float32) * scale
        qpos = start + jnp.arange(MOBA_Q_CHUNK)
        kpos = own * MOBA_BLOCK + jnp.arange(MOBA_BLOCK)
        s_own = jnp.where(kpos[None, :] <= qpos[:, None], s_own, -jnp.inf)
        s = jnp.concatenate([s_sel.reshape(Bn, H, MOBA_Q_CHUNK, ksel * MOBA_BLOCK), s_own], axis=-1)
        p = jax.nn.softmax(s, axis=-1).astype(v.dtype)
        p_sel = p[..., :ksel * MOBA_BLOCK].reshape(Bn, H, MOBA_Q_CHUNK, ksel, MOBA_BLOCK)
        p_own = p[..., ksel * MOBA_BLOCK:]
        return (jnp.einsum('bhqnk,bhqnkd->bhqd', p_sel, v_sel)
                + jnp.einsum('bhqk,bhkd->bhqd', p_own, v_own))

    o = lax.map(chunk, (qc, jnp.arange(nc)))
    return o.transpose(1, 0, 3, 2, 4).reshape(Bn, S, H, D)


def swa_sink_attention(q, k, v, sinks):
    Bn, S, Hq, D = q.shape
    Hkv = k.shape[2]
    G = Hq // Hkv
    scale = HEAD_DIM ** -0.5
    nblk = S // WINDOW
    qb = q.reshape(Bn, nblk, WINDOW, Hkv, G, D)
    kb = k.reshape(Bn, nblk, WINDOW, Hkv, D)
    vb = v.reshape(Bn, nblk, WINDOW, Hkv, D)
    pad_cfg = ((0, 0), (1, 0), (0, 0), (0, 0), (0, 0))
    kk = jnp.concatenate([jnp.pad(kb, pad_cfg)[:, :-1], kb], axis=2)
    vv = jnp.concatenate([jnp.pad(vb, pad_cfg)[:, :-1], vb], axis=2)
    s = jnp.einsum('bnqhgd,bnkhd->bnhgqk', qb, kk).astype(jnp.float32) * scale
    qi = jnp.arange(WINDOW)[:, None] + WINDOW
    ki = jnp.arange(2 * WINDOW)[None, :]
    rel = qi - ki
    band = (rel >= 0) & (rel < WINDOW)
    inside = (jnp.arange(nblk)[:, None, None] * WINDOW + ki[None] - WINDOW) >= 0
    mask = band[None] & inside
    s = jnp.where(mask[None, :, None, None], s, -jnp.inf)
    sink = sinks.astype(jnp.float32).reshape(Hkv, G)[None, None, :, :, None, None]
    m = jnp.maximum(jnp.max(s, axis=-1, keepdims=True), sink)
    p = jnp.exp(s - m)
    p = p / (jnp.sum(p, axis=-1, keepdims=True) + jnp.exp(sink - m))
    o = jnp.einsum('bnhgqk,bnkhd->bnqhgd', p.astype(v.dtype), vv)
    return o.reshape(Bn, S, Hq, D)


def diff_moba_mixer(h, cos, sin, w_in, lambda_q1, lambda_k1, lambda_q2, lambda_k2, subln_g, w_out, lam_init):
    Bn, S, _ = h.shape
    proj = h @ w_in
    splits = [A_WIDTH, 2 * A_WIDTH, 3 * A_WIDTH, 3 * A_WIDTH + B_WIDTH, 3 * A_WIDTH + 2 * B_WIDTH]
    aq, ak, av, bq, bk, bv = jnp.split(proj, splits, axis=-1)
    aq = apply_partial_rope(aq.reshape(Bn, S, 2 * A_HEADS, HEAD_DIM), cos, sin).reshape(Bn, S, A_HEADS, 2, HEAD_DIM)
    ak = apply_partial_rope(ak.reshape(Bn, S, 2 * A_HEADS, HEAD_DIM), cos, sin).reshape(Bn, S, A_HEADS, 2, HEAD_DIM)
    av = av.reshape(Bn, S, A_HEADS, 2 * HEAD_DIM)
    lam = (jnp.exp(jnp.sum(lambda_q1.astype(jnp.float32) * lambda_k1.astype(jnp.float32)))
           - jnp.exp(jnp.sum(lambda_q2.astype(jnp.float32) * lambda_k2.astype(jnp.float32))) + lam_init)
    o_a = diff_attention(aq, ak, av, lam, subln_g, lam_init).reshape(Bn, S, A_WIDTH)
    bq = apply_partial_rope(bq.reshape(Bn, S, B_HEADS, HEAD_DIM), cos, sin)
    bk = apply_partial_rope(bk.reshape(Bn, S, B_HEADS, HEAD_DIM), cos, sin)
    bv = bv.reshape(Bn, S, B_HEADS, HEAD_DIM)
    o_b = moba_attention(bq, bk, bv).reshape(Bn, S, B_WIDTH)
    return jnp.concatenate([o_a, o_b], axis=-1) @ w_out


def swa_mixer(h, cos, sin, w_in, b_in, sinks, w_out):
    Bn, S, _ = h.shape
    proj = h @ w_in + b_in
    q, k, v = jnp.split(proj, [C_Q_WIDTH, C_Q_WIDTH + C_KV_WIDTH], axis=-1)
    q = apply_partial_rope(q.reshape(Bn, S, C_HEADS, HEAD_DIM), cos, sin)
    k = apply_partial_rope(k.reshape(Bn, S, C_KV_HEADS, HEAD_DIM), cos, sin)
    v = v.reshape(Bn, S, C_KV_HEADS, HEAD_DIM)
    return swa_sink_attention(q, k, v, sinks).reshape(Bn, S, C_Q_WIDTH) @ w_out


def grouped_moe(h, router_w, router_bias, w_gate, w_up, w_down):
    Bn, S, D = h.shape
    t = h.reshape(Bn * S, D)
    scores = jax.nn.sigmoid((t @ router_w).astype(jnp.float32))
    biased = scores + router_bias.astype(jnp.float32)
    gscore = jnp.sum(lax.top_k(biased.reshape(-1, N_GROUPS, EXPERTS_PER_GROUP), 2)[0], axis=-1)
    _, gidx = lax.top_k(gscore, TOPK_GROUPS)
    gmask = jnp.any(gidx[..., None] == jnp.arange(N_GROUPS), axis=-2)
    emask = jnp.repeat(gmask, EXPERTS_PER_GROUP, axis=-1)
    _, eidx = lax.top_k(jnp.where(emask, biased, -jnp.inf), TOPK_EXPERTS)
    w = jnp.take_along_axis(scores, eidx, axis=-1)
    w = w / jnp.sum(w, axis=-1, keepdims=True)
    combine = jnp.sum(jax.nn.one_hot(eidx, N_EXPERTS, dtype=jnp.float32) * w[..., None], axis=1)
    out = jnp.zeros(t.shape, jnp.float32)
    for e in range(N_EXPERTS):
        he = jax.nn.silu(t @ w_gate[e]) * (t @ w_up[e])
        out = out + combine[:, e:e + 1] * (he @ w_down[e]).astype(jnp.float32)
    return out.astype(h.dtype).reshape(Bn, S, D)


def setup_inputs(seed: int = 0) -> dict:
    key = jax.random.key(seed)
    ks = iter(jax.random.split(key, 48))
    D = D_MODEL

    def nrm(shape, scale):
        return jax.random.normal(next(ks), shape, jnp.float32) * scale

    def gain(n):
        return 1.0 + nrm((n,), 0.02)

    x = nrm((BATCH, SEQ, D), 1.0)
    c = nrm((BATCH, D), 1.0)
    offsets = jax.random.randint(next(ks), (BATCH, 1), 0, 1024, dtype=jnp.int32)
    positions = (offsets + jnp.arange(SEQ, dtype=jnp.int32)[None, :]).astype(jnp.int32)
    router_w = nrm((D, N_EXPERTS), D ** -0.5)
    router_bias = nrm((N_EXPERTS,), 0.01)
    ada_scale = 0.5 * D ** -0.5

    def experts():
        return (nrm((N_EXPERTS, D, D_FF_EXPERT), D ** -0.5),
                nrm((N_EXPERTS, D, D_FF_EXPERT), D ** -0.5),
                nrm((N_EXPERTS, D_FF_EXPERT, D), D_FF_EXPERT ** -0.5 * DN_BETA))

    col0 = jnp.concatenate([jnp.ones((2 * A_WIDTH,), jnp.float32), jnp.full((A_WIDTH,), DN_BETA, jnp.float32),
                            jnp.ones((2 * B_WIDTH,), jnp.float32), jnp.full((B_WIDTH,), DN_BETA, jnp.float32)])
    l0_w_ada = nrm((D, 6 * D), ada_scale)
    l0_b_ada = nrm((6 * D,), 0.02)
    l0_w_in = nrm((D, 3 * A_WIDTH + 3 * B_WIDTH), D ** -0.5) * col0
    l0_lambda_q1 = nrm((HEAD_DIM,), 0.1)
    l0_lambda_k1 = nrm((HEAD_DIM,), 0.1)
    l0_lambda_q2 = nrm((HEAD_DIM,), 0.1)
    l0_lambda_k2 = nrm((HEAD_DIM,), 0.1)
    l0_subln_g = gain(2 * HEAD_DIM)
    l0_w_out = nrm((A_WIDTH + B_WIDTH, D), (A_WIDTH + B_WIDTH) ** -0.5 * DN_BETA)
    l0_ln1_g = gain(D)
    l0_ln1_b = nrm((D,), 0.02)
    l0_w_gate, l0_w_up, l0_w_down = experts()
    l0_ln2_g = gain(D)
    l0_ln2_b = nrm((D,), 0.02)
    col1 = jnp.concatenate([jnp.ones((C_Q_WIDTH + C_KV_WIDTH,), jnp.float32),
                            jnp.full((C_KV_WIDTH,), DN_BETA, jnp.float32)])
    l1_w_ada = nrm((D, 6 * D), ada_scale)
    l1_b_ada = nrm((6 * D,), 0.02)
    l1_w_in = nrm((D, C_Q_WIDTH + 2 * C_KV_WIDTH), D ** -0.5) * col1
    l1_b_in = nrm((C_Q_WIDTH + 2 * C_KV_WIDTH,), 0.02)
    l1_sinks = nrm((C_HEADS,), 0.5)
    l1_w_out = nrm((C_Q_WIDTH, D), C_Q_WIDTH ** -0.5 * DN_BETA)
    l1_ln1_g = gain(D)
    l1_ln1_b = nrm((D,), 0.02)
    l1_w_gate, l1_w_up, l1_w_down = experts()
    l1_ln2_g = gain(D)
    l1_ln2_b = nrm((D,), 0.02)
    return {
        "x": x, "c": c, "positions": positions,
        "router_w": router_w, "router_bias": router_bias,
        "l0_w_ada": l0_w_ada, "l0_b_ada": l0_b_ada, "l0_w_in": l0_w_in,
        "l0_lambda_q1": l0_lambda_q1, "l0_lambda_k1": l0_lambda_k1,
        "l0_lambda_q2": l0_lambda_q2, "l0_lambda_k2": l0_lambda_k2,
        "l0_subln_g": l0_subln_g, "l0_w_out": l0_w_out,
        "l0_ln1_g": l0_ln1_g, "l0_ln1_b": l0_ln1_b,
        "l0_w_gate": l0_w_gate, "l0_w_up": l0_w_up, "l0_w_down": l0_w_down,
        "l0_ln2_g": l0_ln2_g, "l0_ln2_b": l0_ln2_b,
        "l1_w_ada": l1_w_ada, "l1_b_ada": l1_b_ada, "l1_w_in": l1_w_in, "l1_b_in": l1_b_in,
        "l1_sinks": l1_sinks, "l1_w_out": l1_w_out,
        "l1_ln1_g": l1_ln1_g, "l1_ln1_b": l1_ln1_b,
        "l1_w_gate": l1_w_gate, "l1_w_up": l1_w_up, "l1_w_down": l1_w_down,
        "l1_ln2_g": l1_ln2_g, "l1_ln2_b": l1_ln2_b,
    }


def reference(x, c, positions, router_w, router_bias,
              l0_w_ada, l0_b_ada, l0_w_in, l0_lambda_q1, l0_lambda_k1, l0_lambda_q2, l0_lambda_k2,
              l0_subln_g, l0_w_out, l0_ln1_g, l0_ln1_b, l0_w_gate, l0_w_up, l0_w_down, l0_ln2_g, l0_ln2_b,
              l1_w_ada, l1_b_ada, l1_w_in, l1_b_in, l1_sinks, l1_w_out, l1_ln1_g, l1_ln1_b,
              l1_w_gate, l1_w_up, l1_w_down, l1_ln2_g, l1_ln2_b):
    cos, sin = rope_tables(positions)
    ada = [(l0_w_ada, l0_b_ada), (l1_w_ada, l1_b_ada)]
    post = [(l0_ln1_g, l0_ln1_b, l0_ln2_g, l0_ln2_b), (l1_ln1_g, l1_ln1_b, l1_ln2_g, l1_ln2_b)]
    ffn = [(l0_w_gate, l0_w_up, l0_w_down), (l1_w_gate, l1_w_up, l1_w_down)]
    for layer in range(DEPTH):
        shift1, scale1, gate1, shift2, scale2, gate2 = adaln_modulation(c, ada[layer][0], ada[layer][1])
        ln1_g, ln1_b, ln2_g, ln2_b = post[layer]
        h = x * (1.0 + scale1) + shift1
        if layer % 2 == 0:
            lam_init = 0.8 - 0.6 * math.exp(-0.3 * layer)
            y = diff_moba_mixer(h, cos, sin, l0_w_in, l0_lambda_q1, l0_lambda_k1, l0_lambda_q2, l0_lambda_k2,
                                l0_subln_g, l0_w_out, lam_init)
        else:
            y = swa_mixer(h, cos, sin, l1_w_in, l1_b_in, l1_sinks, l1_w_out)
        x = layer_norm(DN_ALPHA * x + gate1 * y, ln1_g, ln1_b)
        h = x * (1.0 + scale2) + shift2
        y = grouped_moe(h, router_w, router_bias, ffn[layer][0], ffn[layer][1], ffn[layer][2])
        x = layer_norm(DN_ALPHA * x + gate2 * y, ln2_g, ln2_b)
    return x
```

```python
import contextlib
import math
import numpy as np
import concourse.bass as bass
import concourse.mybir as mybir
from concourse.bass_utils import run_bass_kernel_spmd

F32 = mybir.dt.float32
BF16 = mybir.dt.bfloat16
I32 = mybir.dt.int32
ALU = mybir.AluOpType
AF = mybir.ActivationFunctionType
AX = mybir.AxisListType

D = 4096
KC = D // 128
HD = 128
NE = 16
DFF = 1024
BIG = 30000.0
ALPHA = float(4 ** 0.25)
LN_EPS = 1e-5
RMS_EPS = 1e-5
LAM_INIT = 0.8 - 0.6 * math.exp(0.0)
SCALE = HD ** -0.5
PI = math.pi

ENGS = ("pe", "act", "dve", "pool", "sp")


class Op:
    __slots__ = ("eng", "fn", "deps", "marked", "cnt", "is_dma", "dsem", "dcnt", "prev_dcnt")

    def __init__(self, eng, fn, is_dma):
        self.eng = eng
        self.fn = fn
        self.deps = ()
        self.marked = False
        self.cnt = 0
        self.is_dma = is_dma
        self.dsem = -1
        self.dcnt = 0
        self.prev_dcnt = 0


class Prog:
    def __init__(self, nc, n_dma_sems=32):
        self.nc = nc
        self.q = {e: [] for e in ENGS}
        self.st = {}
        self.n_dma_sems = n_dma_sems
        self.dma_order = []

    def add(self, eng, fn, r=(), w=(), dma=False):
        op = Op(eng, fn, dma)
        st = self.st
        deps = set()
        for k in r:
            s = st.get(k)
            if s is not None and s[0] is not None:
                deps.add(s[0])
        for k in w:
            s = st.get(k)
            if s is not None:
                if s[0] is not None:
                    deps.add(s[0])
                for x in s[1]:
                    deps.add(x)
        for k in r:
            s = st.get(k)
            if s is None:
                st[k] = [None, [op]]
            else:
                s[1].append(op)
        for k in w:
            st[k] = [op, []]
        deps.discard(op)
        if not dma and eng == "pe":
            deps = [d for d in deps if d.is_dma or d.eng != "pe"]
        op.deps = tuple(deps)
        self.q[eng].append(op)
        if dma:
            self.dma_order.append(op)
        return op

    def dma(self, eng, out, in_, r=(), w=(), **kw):
        return self.add(eng, lambda e: e.dma_start(out=out, in_=in_, **kw), r, w, dma=True)

    def mm(self, out, lhsT, rhs, start, stop, r=(), w=()):
        return self.add("pe", lambda e: e.matmul(out, lhsT, rhs, start=start, stop=stop), r, w)

    def tr(self, out, in_, ident, r=(), w=()):
        return self.add("pe", lambda e: e.transpose(out, in_, ident), r, w)

    def emit(self):
        nc = self.nc
        for e in ENGS:
            for op in self.q[e]:
                for d in op.deps:
                    d.marked = True
        for e in ENGS:
            c = 0
            for op in self.q[e]:
                if not op.is_dma and op.marked:
                    c += 1
                op.cnt = c
        uses = [0] * self.n_dma_sems
        for k, op in enumerate(self.dma_order):
            s = k % self.n_dma_sems
            op.dsem = s
            op.prev_dcnt = 16 * uses[s]
            uses[s] += 1
            op.dcnt = 16 * uses[s]
        final_dma = [16 * u for u in uses]

        with contextlib.ExitStack() as es:
            esem = {e: es.enter_context(nc.semaphore("s_" + e)) for e in ENGS}
            dsems = [es.enter_context(nc.semaphore("d_%d" % i)) for i in range(self.n_dma_sems)]
            block = es.enter_context(nc.Block())

            def run(e, eng):
                seen = {}
                for op in self.q[e]:
                    for d in op.deps:
                        if d.is_dma:
                            key, thr, sem = ("d", d.dsem), d.dcnt, dsems[d.dsem]
                        else:
                            key, thr, sem = ("e", d.eng), d.cnt, esem[d.eng]
                        if seen.get(key, 0) < thr:
                            eng.wait_ge(sem, thr)
                            seen[key] = thr
                    if op.is_dma:
                        key = ("d", op.dsem)
                        if op.prev_dcnt > 0 and seen.get(key, 0) < op.prev_dcnt:
                            eng.wait_ge(dsems[op.dsem], op.prev_dcnt)
                            seen[key] = op.prev_dcnt
                        op.fn(eng).then_inc(dsems[op.dsem], 16)
                    else:
                        ins = op.fn(eng)
                        if op.marked:
                            ins.then_inc(esem[e], 1)
                if e == "sp":
                    for i, v in enumerate(final_dma):
                        if v > 0:
                            eng.wait_ge(dsems[i], v)

            block.tensor(lambda eng: run("pe", eng))
            block.scalar(lambda eng: run("act", eng))
            block.vector(lambda eng: run("dve", eng))
            block.gpsimd(lambda eng: run("pool", eng))
            block.sync(lambda eng: run("sp", eng))


class Ctx:
    def __init__(self):
        self.nc = bass.Bass("TRN2", target_bir_lowering=False)
        self.es = contextlib.ExitStack()
        self.P = Prog(self.nc)

    def din(self, name, shape, dt):
        return self.nc.dram_tensor(name, list(shape), dt, kind="ExternalInput").ap()

    def dout(self, name, shape, dt):
        return self.nc.dram_tensor(name, list(shape), dt, kind="ExternalOutput").ap()

    def dscr(self, name, shape, dt):
        return self.nc.dram_tensor(name, list(shape), dt, kind="Internal").ap()

    def sb(self, name, shape, dt):
        return self.es.enter_context(self.nc.sbuf_tensor(name, list(shape), dt))

    def ps(self, name, shape, dt=F32):
        return self.es.enter_context(self.nc.psum_tensor(name, list(shape), dt))

    def finish(self):
        self.P.emit()
        self.es.close()
        return self.nc


def _consts():
    ident = np.eye(128, dtype=np.float32)
    r32t = np.zeros((32, 32), np.float32)
    for i in range(16):
        r32t[i + 16, i] = -1.0
        r32t[i, i + 16] = 1.0
    inv = (500000.0 ** (-np.arange(0, 32, 2, dtype=np.float32) / 32.0)).astype(np.float32)
    invf = np.concatenate([inv, inv]).reshape(32, 1).astype(np.float32)
    return ident, r32t, invf


def build_ada(ncol):
    C = Ctx()
    P = C.P
    c_in = C.din("c", [2, D], F32)
    w0 = C.din("w0", [D, ncol], F32)
    b0 = C.din("b0", [1, ncol], F32)
    w1 = C.din("w1", [D, ncol], F32)
    b1 = C.din("b1", [1, ncol], F32)
    m0 = C.dout("m0", [2, ncol], F32)
    m1 = C.dout("m1", [2, ncol], F32)
    cT = C.sb("cT", [128, KC, 2], F32)
    sT = C.sb("sT", [128, KC, 2], F32)
    wk = [C.sb("wk%d" % i, [128, ncol], F32) for i in range(3)]
    bb = C.sb("bb", [2, ncol], F32)
    res = C.sb("res", [2, ncol], F32)
    nch = ncol // 512
    pm = [C.ps("pm%d" % i, [2, 512]) for i in range(nch)]
    for b_ in range(2):
        P.dma("sp", cT[:, :, b_], c_in[b_, :].rearrange("(c p) -> p c", p=128), w=["cT"], allow_slow_non_contiguous=True)
    P.add("act", lambda e: e.activation(sT[:], cT[:], AF.Silu), r=["cT"], w=["sT"])
    for li, (w, b, m) in enumerate(((w0, b0, m0), (w1, b1, m1))):
        for kc in range(KC):
            s = (li * KC + kc) % 3
            P.dma("sp", wk[s][:], w[kc * 128:(kc + 1) * 128, :], w=[("wk", s)])
            for n in range(nch):
                P.mm(pm[n][:], sT[:, kc, :], wk[s][:, n * 512:(n + 1) * 512], kc == 0, kc == KC - 1,
                     r=["sT", ("wk", s)], w=[("pm", n)])
        for row in range(2):
            P.dma("sp", bb[row:row + 1, :], b, w=[("bb", row)])
        for n in range(nch):
            P.add("dve", lambda e, n=n: e.tensor_tensor(res[:, n * 512:(n + 1) * 512], pm[n][:],
                                                        bb[:, n * 512:(n + 1) * 512], ALU.add),
                  r=[("pm", n), ("bb", 0), ("bb", 1)], w=[("res", n)])
        P.dma("sp", m, res[:], r=[("res", n) for n in range(nch)])
    return C.finish()


class TokStage:
    def __init__(self, Tc, parts):
        self.Tc = Tc
        self.Tb = min(512, Tc)
        self.nt = self.Tb // 128
        self.nblk = Tc // self.Tb
        self.parts = parts
        self.C = Ctx()
        self.build()

    def wload(self, src_ap, shape3):
        s = self.wnext % self.nslots
        self.wnext += 1
        a, b = shape3
        view = self.wb[s][:, 0:a * b].rearrange("p (a b) -> p a b", b=b)
        self.C.P.dma("pool", view, src_ap, w=[("wb", s)])
        return view, ("wb", s)

    def pipeline(self, units, depth=3):
        loaded = []
        n = len(units)
        for i in range(n + depth):
            if i < n:
                loaded.append(units[i][0]())
            j = i - depth
            if j >= 0:
                units[j][1](*loaded[j])
                loaded[j] = None

    def load_vec_bc(self, slot, src_row):
        wk = [("vb", slot)] + ([("ob", 0), ("ob", 1)] if slot == 1 else [])
        self.C.P.dma("sp", self.vb[slot][:], src_row.partition_broadcast(128), w=wk)

    def load_vec_fm(self, dst, key, src_row):
        self.C.P.dma("sp", dst[:], src_row.rearrange("(c p) -> p c", p=128), w=[key],
                     allow_slow_non_contiguous=True)

    def transpose_modulate(self, scp, shf, keys):
        P = self.C.P
        nt = self.nt
        g = 0
        for t in range(nt):
            for k4 in range(KC // 4):
                pb = self.pT[g % 2]
                pk = ("pT", g % 2)
                g += 1
                for j in range(4):
                    kc = k4 * 4 + j
                    P.tr(pb[:, j, :], self.xres[:, t, kc * 128:(kc + 1) * 128], self.identf[:],
                         r=[("xres", t), "identf"], w=[pk])
                for j in range(4):
                    kc = k4 * 4 + j
                    P.add("act", lambda e, pb=pb, j=j, kc=kc, t=t: e.activation(
                        self.actT[:, kc, t * 128:(t + 1) * 128], pb[:, j, :], AF.Identity,
                        bias=shf[:, kc:kc + 1], scale=scp[:, kc:kc + 1]),
                        r=[pk] + keys, w=[("actT", t)])

    def sin_turns(self, dst, dkey, off):
        P = self.C.P
        rt, ri = self.rtmp, self.rint
        P.add("dve", lambda e: e.tensor_scalar(rt[:], self.ang[:], 1.0 / (2 * PI), off, ALU.mult, ALU.add),
              r=["ang"], w=["rtmp"])
        P.add("dve", lambda e: e.tensor_copy(ri[:], rt[:]), r=["rtmp"], w=["rint"])
        P.add("dve", lambda e: e.tensor_copy(self.rflt[:], ri[:]), r=["rint"], w=["rflt"])
        P.add("dve", lambda e: e.tensor_tensor(rt[:], rt[:], self.rflt[:], ALU.subtract), r=["rtmp", "rflt"], w=["rtmp"])
        P.add("dve", lambda e: e.scalar_tensor_tensor(self.rflt[:], rt[:], 0.0, rt[:], ALU.is_lt, ALU.add),
              r=["rtmp"], w=["rflt"])
        P.add("dve", lambda e: e.tensor_scalar(rt[:], self.rflt[:], 2 * PI, -PI, ALU.mult, ALU.add), r=["rflt"], w=["rtmp"])
        P.add("dve", lambda e: e.tensor_scalar(rt[:], rt[:], 3.14159, -3.14159, ALU.min, ALU.max), r=["rtmp"], w=["rtmp"])
        P.add("act", lambda e: e.activation(dst[:], rt[:], AF.Sin), r=["rtmp"], w=[dkey])

    def layer_norm(self, t, g_slot_loader, b_slot_loader):
        P = self.C.P
        st = self.lnst
        for c in range(8):
            P.add("dve", lambda e, c=c: e.bn_stats(st[:, c, :], self.xres[:, t, c * 512:(c + 1) * 512]),
                  r=[("xres", t)], w=["lnst"])
        P.add("dve", lambda e: e.bn_aggr(self.mv[:], st[:].rearrange("p a b -> p (a b)")), r=["lnst"], w=["mv"])
        P.add("dve", lambda e: e.tensor_scalar_add(self.rstd[:], self.mv[:, 1:2], LN_EPS), r=["mv"], w=["rstd"])
        P.add("dve", lambda e: e.reciprocal(self.rstd[:], self.rstd[:]), r=["rstd"], w=["rstd"])
        P.add("act", lambda e: e.activation(self.rstd[:], self.rstd[:], AF.Sqrt), r=["rstd"], w=["rstd"])
        P.add("dve", lambda e: e.tensor_scalar(self.xres[:, t, :], self.xres[:, t, :], self.mv[:, 0:1],
                                               self.rstd[:, 0:1], ALU.subtract, ALU.mult),
              r=["mv", "rstd", ("xres", t)], w=[("xres", t)])

    def build(self):
        C = self.C
        P = C.P
        Tc, Tb, nt = self.Tc, self.Tb, self.nt
        parts = self.parts
        kinds = [p[0] for p in parts]
        layers = sorted(set(p[1] for p in parts))
        has_back = "back" in kinds
        has_front = "front" in kinds
        first_kind = kinds[0]
        io = {}
        io["ident"] = C.din("ident", [128, 128], F32)
        io["xin"] = C.din("xin", [Tc, D], F32)
        for L in layers:
            io["mod%d" % L] = C.din("mod%d" % L, [6, D], F32)
        if has_back:
            Lb = [p[1] for p in parts if p[0] == "back"][0]
            self.Lb = Lb
            io["oin"] = C.din("oin", [Tc, D], BF16)
            io["w_out"] = C.din("w_out", [D, D], F32)
            io["ln"] = C.din("ln", [4, D], F32)
            io["router_w"] = C.din("router_w", [D, NE], F32)
            io["router_b"] = C.din("router_b", [1, NE], F32)
            io["w_gate"] = C.din("w_gate", [NE, D, DFF], F32)
            io["w_up"] = C.din("w_up", [NE, D, DFF], F32)
            io["w_down"] = C.din("w_down", [NE, DFF, D], F32)
            io["xout"] = C.dout("xout", [Tc, D], F32)
            io["x1scr"] = C.dscr("x1scr", [Tc, D], F32)
        if has_front:
            Lf = [p[1] for p in parts if p[0] == "front"][0]
            self.Lf = Lf
            ncols = 12288 if Lf == 0 else 6144
            self.fm_blocks = (list(range(0, 32)) + list(range(48, 80))) if Lf == 0 else list(range(0, 40))
            self.v_blocks = (list(range(32, 48)) + list(range(80, 96))) if Lf == 0 else list(range(40, 48))
            io["w_in"] = C.din("w_in", [D, ncols], F32)
            if Lf == 1:
                io["b_in"] = C.din("b_in", [1, ncols], F32)
            io["pos"] = C.din("pos", [1, Tc], I32)
            io["r32t"] = C.din("r32t", [32, 32], F32)
            io["invf"] = C.din("invf", [32, 1], F32)
            io["qkT"] = C.dout("qkT", [len(self.fm_blocks), 128, Tc], BF16)
            io["vout"] = C.dout("vout", [Tc, len(self.v_blocks) * 128], BF16)
        self.io = io
        self.xres = C.sb("xres", [128, nt, D], F32)
        self.actT = C.sb("actT", [128, KC, Tb], BF16)
        self.nslots = 4
        self.wb = [C.sb("wb%d" % i, [128, 4096], BF16) for i in range(self.nslots)]
        self.wnext = 0
        self.vb = [C.sb("vb%d" % i, [128, D], F32) for i in range(2)]
        self.identf = C.sb("identf", [128, 128], F32)
        self.identb = C.sb("identb", [128, 128], BF16)
        self.lnst = C.sb("lnst", [128, 8, 6], F32)
        self.mv = C.sb("mv", [128, 2], F32)
        self.rstd = C.sb("rstd", [128, 1], F32)
        self.fmv = {}
        for L in layers:
            for nm in ("sc1", "sh1", "sc2", "sh2"):
                self.fmv[(L, nm)] = C.sb("fm_%s_%d" % (nm, L), [128, KC], F32)
        self.pT = [C.ps("pT%d" % i, [128, 4, 128]) for i in range(2)]
        self.pA = [C.ps("pA%d" % i, [128, 512]) for i in range(2)]
        self.pB = [C.ps("pB%d" % i, [128, 512]) for i in range(2)]
        self.pD = [C.ps("pD%d" % i, [128, 512]) for i in range(2)]
        if has_back:
            vb1b = self.vb[1][:].bitcast(BF16)
            self.ob = [vb1b[:, 0:D], vb1b[:, D:2 * D]]
            self.heT = C.sb("heT", [128, 8, Tb], BF16)
            self.sg = [C.sb("sg%d" % i, [128, Tb], F32) for i in range(2)]
            self.rw = C.sb("rw", [128, KC, NE], BF16)
            self.rb = C.sb("rb", [128, NE], F32)
            self.comb = C.sb("comb", [128, nt, NE], F32)
            self.rt = {nm: C.sb("rt_" + nm, [128, NE], F32) for nm in ("sc", "bi", "mk", "w")}
            self.rs = {nm: C.sb("rs_" + nm, [128, 8], F32) for nm in ("m1", "m2", "eq", "b2", "gs", "gm", "gk", "pen", "top", "ws")}
            self.tmp = self.sg
        if has_front:
            self.r32 = C.sb("r32", [32, 32], F32)
            self.invf = C.sb("invf_s", [32, 1], F32)
            self.posi = C.sb("posi", [32, Tb], I32)
            self.ang = C.sb("ang", [32, Tb], F32)
            self.rtmp = C.sb("rtmp", [32, Tb], F32)
            self.rint = C.sb("rint", [32, Tb], I32)
            self.rflt = C.sb("rflt", [32, Tb], F32)
            self.cosT = C.sb("cosT", [32, Tb], F32)
            self.sinT = C.sb("sinT", [32, Tb], F32)
            self.xf32 = [C.sb("xf32_0", [32, Tb], F32)] * 2
            self.t1 = [C.sb("t1_0", [32, Tb], F32)] * 2
            self.qst = [C.sb("qst%d" % i, [128, Tb], BF16) for i in range(2)]
            self.vst = [C.sb("vst%d" % i, [128, nt, 128], BF16) for i in range(2)]
            if Lf == 1:
                self.binT = C.sb("binT", [128, len(self.fm_blocks)], F32)
                self.binV = C.sb("binV", [128, len(self.v_blocks) * 128], F32)
        P.dma("sp", self.identf[:], io["ident"], w=["identf"])
        P.add("dve", lambda e: e.tensor_copy(self.identb[:], self.identf[:]), r=["identf"], w=["identb"])
        for L in layers:
            mod = io["mod%d" % L]
            for nm, row in (("sh1", 0), ("sc1", 1), ("sh2", 3), ("sc2", 4)):
                t = self.fmv[(L, nm)]
                self.load_vec_fm(t, ("fmv", L, nm), mod[row, :])
                if nm.startswith("sc"):
                    P.add("dve", lambda e, t=t: e.tensor_scalar_add(t[:], t[:], 1.0),
                          r=[("fmv", L, nm)], w=[("fmv", L, nm)])
        if has_back:
            P.dma("pool", self.rw[:], io["router_w"].rearrange("(c p) n -> p c n", p=128), w=["rw"],
                  allow_slow_non_contiguous=True)
            P.dma("sp", self.rb[:], io["router_b"][0, :].partition_broadcast(128), w=["rb"])
        if has_front:
            P.dma("sp", self.r32[:], io["r32t"], w=["r32"])
            P.dma("sp", self.invf[:], io["invf"], w=["invf"])
            if self.Lf == 1:
                P.dma("sp", self.binT[:], io["b_in"][0, 0:len(self.fm_blocks) * 128].rearrange("(c p) -> p c", p=128),
                      w=["binT"], allow_slow_non_contiguous=True)
                P.dma("sp", self.binV[:], io["b_in"][0, 5120:6144].partition_broadcast(128), w=["binV"])
        for blk in range(self.nblk):
            t0 = blk * Tb
            x_in_sbuf = False
            for kind, L in parts:
                if kind == "back":
                    self.back(L, t0)
                    x_in_sbuf = True
                else:
                    self.front(L, t0, x_in_sbuf)
        self.nc = C.finish()

    def front(self, L, t0, x_in_sbuf):
        C, P, io = self.C, self.C.P, self.io
        Tb, nt = self.Tb, self.nt
        if not x_in_sbuf:
            for t in range(nt):
                P.dma("sp", self.xres[:, t, :], io["xin"][t0 + t * 128:t0 + (t + 1) * 128, :], w=[("xres", t)])
        self.transpose_modulate(self.fmv[(L, "sc1")], self.fmv[(L, "sh1")], [("fmv", L, "sc1"), ("fmv", L, "sh1")])
        P.dma("sp", self.posi[:], io["pos"][0, t0:t0 + Tb].partition_broadcast(32), w=["posi"])
        P.add("dve", lambda e: e.tensor_copy(self.ang[:], self.posi[:]), r=["posi"], w=["ang"])
        P.add("dve", lambda e: e.tensor_scalar(self.ang[:], self.ang[:], self.invf[:, 0:1], None, ALU.mult),
              r=["ang", "invf"], w=["ang"])
        self.sin_turns(self.sinT, "sinT", 0.5)
        self.sin_turns(self.cosT, "cosT", 0.75)
        w_in = io["w_in"]
        use_bias = (L == 1)
        units = []
        cnt = [0]

        def fm_unit(bi, cb):
            def load():
                return self.wload(w_in[:, cb * 128:(cb + 1) * 128].rearrange("(c p) n -> p c n", p=128), (KC, 128))

            def comp(wv, wk):
                i = cnt[0]
                cnt[0] += 1
                pq = self.pA[i % 2]
                pk = ("pA", i % 2)
                for kc in range(KC):
                    P.mm(pq[:, 0:Tb], wv[:, kc, :], self.actT[:, kc, :], kc == 0, kc == KC - 1,
                         r=[wk] + [("actT", t) for t in range(nt)], w=[pk])
                qs = self.qst[i % 2]
                qk = ("qst", i % 2)
                xf = self.xf32[i % 2]
                xk = ("xf32", 0)
                t1 = self.t1[i % 2]
                tk = ("t1", 0)
                pr = self.pD[i % 2]
                prk = ("pD", i % 2)
                if use_bias:
                    bia = self.binT[:, bi:bi + 1]
                    bia32 = self.binT[0:32, bi:bi + 1]
                    rk = ["binT"]
                else:
                    bia, bia32, rk = 0.0, 0.0, []
                P.add("act", lambda e: e.activation(qs[:, 0:Tb], pq[:, 0:Tb], AF.Identity, bias=bia),
                      r=[pk] + rk, w=[qk])
                P.add("act", lambda e: e.activation(xf[:], pq[0:32, 0:Tb], AF.Identity, bias=bia32),
                      r=[pk] + rk, w=[xk])
                P.mm(pr[0:32, 0:Tb], self.r32[:], xf[:], True, True, r=["r32", xk], w=[prk])
                P.add("dve", lambda e: e.tensor_tensor(t1[:], xf[:], self.cosT[:], ALU.mult), r=[xk, "cosT"], w=[tk])
                P.add("dve", lambda e: e.tensor_tensor(xf[:], pr[0:32, 0:Tb], self.sinT[:], ALU.mult),
                      r=[prk, "sinT"], w=[xk])
                P.add("dve", lambda e: e.tensor_tensor(qs[0:32, 0:Tb], t1[:], xf[:], ALU.add), r=[tk, xk, qk], w=[qk])
                P.dma("sp", io["qkT"][bi, :, t0:t0 + Tb], qs[:, 0:Tb], r=[qk])
            return (load, comp)

        def v_unit(vi, cb):
            def load():
                return self.wload(w_in[:, cb * 128:(cb + 1) * 128].rearrange("(c p) n -> p c n", p=128), (KC, 128))

            def comp(wv, wk):
                i = cnt[0]
                cnt[0] += 1
                pv = self.pB[i % 2]
                pk = ("pB", i % 2)
                vs = self.vst[i % 2]
                vk = ("vst", i % 2)
                for t in range(nt):
                    for kc in range(KC):
                        P.mm(pv[:, t * 128:(t + 1) * 128], self.actT[:, kc, t * 128:(t + 1) * 128], wv[:, kc, :],
                             kc == 0, kc == KC - 1, r=[wk, ("actT", t)], w=[pk])
                for t in range(nt):
                    if use_bias:
                        P.add("dve", lambda e, t=t: e.tensor_tensor(vs[:, t, :], pv[:, t * 128:(t + 1) * 128],
                                                                    self.binV[:, vi * 128:(vi + 1) * 128], ALU.add),
                              r=[pk, "binV"], w=[vk])
                    else:
                        P.add("dve", lambda e, t=t: e.tensor_copy(vs[:, t, :], pv[:, t * 128:(t + 1) * 128]), r=[pk], w=[vk])
                P.dma("sp", io["vout"][t0:t0 + Tb, vi * 128:(vi + 1) * 128].rearrange("(t p) n -> p t n", p=128),
                      vs[:], r=[vk])
            return (load, comp)

        for bi, cb in enumerate(self.fm_blocks):
            units.append(fm_unit(bi, cb))
        for vi, cb in enumerate(self.v_blocks):
            units.append(v_unit(vi, cb))
        self.pipeline(units)

    def router(self, t):
        P = self.C.P
        pr = self.pT[0]
        prk = ("pT", 0)
        lg = pr[:, 0, 0:NE]
        for kc in range(KC):
            P.mm(lg, self.actT[:, kc, t * 128:(t + 1) * 128], self.rw[:, kc, :], kc == 0, kc == KC - 1,
                 r=[("actT", t), "rw"], w=[prk])
        rt, rs = self.rt, self.rs
        V = lambda nm, a, b: (rt[nm] if nm in rt else rs[nm])[:, a:b]
        seq = []

        def dve(fn, r, w):
            P.add("dve", fn, r=r, w=w)
        dve_keys = lambda *n: ["r_" + x for x in n]
        P.add("act", lambda e: e.activation(rt["sc"][:], lg, AF.Sigmoid), r=[prk], w=["r_sc"])
        dve(lambda e: e.tensor_tensor(rt["bi"][:], rt["sc"][:], self.rb[:], ALU.add), ["r_sc", "rb"], ["r_bi"])
        for g in range(4):
            bg = rt["bi"][:, 4 * g:4 * g + 4]
            dve(lambda e, bg=bg, g=g: e.tensor_reduce(rs["m1"][:, g:g + 1], bg, AX.X, ALU.max), ["r_bi"], ["r_m1"])
            dve(lambda e, bg=bg, g=g: e.tensor_scalar(rs["eq"][:, 0:4], bg, rs["m1"][:, g:g + 1], None, ALU.is_equal),
                ["r_bi", "r_m1"], ["r_eq"])
            dve(lambda e, bg=bg: e.scalar_tensor_tensor(rs["b2"][:, 0:4], rs["eq"][:, 0:4], -BIG, bg, ALU.mult, ALU.add),
                ["r_eq", "r_bi"], ["r_b2"])
            dve(lambda e, g=g: e.tensor_reduce(rs["m2"][:, g:g + 1], rs["b2"][:, 0:4], AX.X, ALU.max), ["r_b2"], ["r_m2"])
        dve(lambda e: e.tensor_tensor(rs["gs"][:, 0:4], rs["m1"][:, 0:4], rs["m2"][:, 0:4], ALU.add), ["r_m1", "r_m2"], ["r_gs"])
        dve(lambda e: e.tensor_reduce(rs["gm"][:, 0:1], rs["gs"][:, 0:4], AX.X, ALU.max), ["r_gs"], ["r_gm"])
        dve(lambda e: e.tensor_scalar(rs["gk"][:, 0:4], rs["gs"][:, 0:4], rs["gm"][:, 0:1], None, ALU.is_equal),
            ["r_gs", "r_gm"], ["r_gk"])
        dve(lambda e: e.tensor_scalar(rs["pen"][:, 0:4], rs["gk"][:, 0:4], 1.0, BIG, ALU.subtract, ALU.mult), ["r_gk"], ["r_pen"])
        for g in range(4):
            dve(lambda e, g=g: e.tensor_scalar(rt["mk"][:, 4 * g:4 * g + 4], rt["bi"][:, 4 * g:4 * g + 4],
                                               rs["pen"][:, g:g + 1], None, ALU.add), ["r_bi", "r_pen"], ["r_mk"])
        dve(lambda e: e.max(rs["top"][:, 0:8], rt["mk"][:]), ["r_mk"], ["r_top"])
        dve(lambda e: e.tensor_scalar(rt["w"][:], rt["mk"][:], rs["top"][:, 1:2], None, ALU.is_ge), ["r_mk", "r_top"], ["r_w"])
        dve(lambda e: e.tensor_tensor(rt["w"][:], rt["w"][:], rt["sc"][:], ALU.mult), ["r_w", "r_sc"], ["r_w"])
        dve(lambda e: e.tensor_reduce(rs["ws"][:, 0:1], rt["w"][:], AX.X, ALU.add), ["r_w"], ["r_ws"])
        dve(lambda e: e.reciprocal(rs["ws"][:, 1:2], rs["ws"][:, 0:1]), ["r_ws"], ["r_ws"])
        dve(lambda e: e.tensor_scalar(self.comb[:, t, :], rt["w"][:], rs["ws"][:, 1:2], None, ALU.mult),
            ["r_w", "r_ws"], [("comb", t)])

    def back(self, L, t0):
        C, P, io = self.C, self.C.P, self.io
        Tb, nt = self.Tb, self.nt
        mod = io["mod%d" % L]
        g = 0
        for t in range(nt):
            ob = self.ob[t % 2]
            P.dma("sp", ob, io["oin"][t0 + t * 128:t0 + (t + 1) * 128, :], w=[("ob", t % 2), ("vb", 1)])
            for k8 in range(KC // 8):
                pb = self.pT[g % 2]
                pk = ("pT", g % 2)
                g += 1
                pbb = pb[:].rearrange("p a b -> p (a b)").bitcast(BF16)
                for j in range(8):
                    kc = k8 * 8 + j
                    P.tr(pbb[:, j * 128:(j + 1) * 128], ob[:, kc * 128:(kc + 1) * 128], self.identb[:],
                         r=[("ob", t % 2), "identb"], w=[pk])
                P.add("act", lambda e, pbb=pbb, k8=k8, t=t: e.activation(
                    self.actT[:, k8 * 8:(k8 + 1) * 8, t * 128:(t + 1) * 128],
                    pbb.rearrange("p (a b) -> p a b", b=128), AF.Identity), r=[pk], w=[("actT", t)])
        for t in range(nt):
            P.dma("sp", self.xres[:, t, :], io["xin"][t0 + t * 128:t0 + (t + 1) * 128, :], w=[("xres", t)])
        self.load_vec_bc(0, mod[2, :])
        w_out = io["w_out"]
        cnt = [0]

        def wo_unit(cb):
            def load():
                return self.wload(w_out[:, cb * 128:(cb + 1) * 128].rearrange("(c p) n -> p c n", p=128), (KC, 128))

            def comp(wv, wk):
                i = cnt[0]
                cnt[0] += 1
                pv = self.pB[i % 2]
                pk = ("pB", i % 2)
                tm = self.tmp[i % 2]
                tk = ("sg", i % 2)
                for t in range(nt):
                    for kc in range(KC):
                        P.mm(pv[:, t * 128:(t + 1) * 128], self.actT[:, kc, t * 128:(t + 1) * 128], wv[:, kc, :],
                             kc == 0, kc == KC - 1, r=[wk, ("actT", t)], w=[pk])
                for t in range(nt):
                    xs = self.xres[:, t, cb * 128:(cb + 1) * 128]
                    P.add("dve", lambda e, t=t: e.tensor_tensor(tm[:, t * 128:(t + 1) * 128], pv[:, t * 128:(t + 1) * 128],
                                                                self.vb[0][:, cb * 128:(cb + 1) * 128], ALU.mult),
                          r=[pk, ("vb", 0)], w=[tk])
                    P.add("dve", lambda e, t=t, xs=xs: e.scalar_tensor_tensor(xs, xs, ALPHA, tm[:, t * 128:(t + 1) * 128],
                                                                              ALU.mult, ALU.add),
                          r=[tk, ("xres", t)], w=[("xres", t)])
            return (load, comp)
        self.pipeline([wo_unit(cb) for cb in range(KC)])
        self.load_vec_bc(1, io["ln"][0, :])
        for t in range(nt):
            self.layer_norm(t, None, None)
            P.add("dve", lambda e, t=t: e.tensor_tensor(self.xres[:, t, :], self.xres[:, t, :], self.vb[1][:], ALU.mult),
                  r=[("xres", t), ("vb", 1)], w=[("xres", t)])
        self.load_vec_bc(0, io["ln"][1, :])
        for t in range(nt):
            P.add("dve", lambda e, t=t: e.tensor_tensor(self.xres[:, t, :], self.xres[:, t, :], self.vb[0][:], ALU.add),
                  r=[("xres", t), ("vb", 0)], w=[("xres", t)])
            P.dma("sp", io["x1scr"][t0 + t * 128:t0 + (t + 1) * 128, :], self.xres[:, t, :], r=[("xres", t)],
                  w=[("x1scr", t0, t)])
        self.transpose_modulate(self.fmv[(L, "sc2")], self.fmv[(L, "sh2")], [("fmv", L, "sc2"), ("fmv", L, "sh2")])
        for t in range(nt):
            self.router(t)
        wg, wu, wd = io["w_gate"], io["w_up"], io["w_down"]
        units = []
        cnt = [0]
        dcnt = [0]

        def gu_unit(e_, fc):
            def load():
                a = self.wload(wg[e_, :, fc * 128:(fc + 1) * 128].rearrange("(c p) n -> p c n", p=128), (KC, 128))
                b = self.wload(wu[e_, :, fc * 128:(fc + 1) * 128].rearrange("(c p) n -> p c n", p=128), (KC, 128))
                return a + b

            def comp(gv, gk, uv, uk):
                i = cnt[0]
                cnt[0] += 1
                pg, pgk = self.pA[i % 2], ("pA", i % 2)
                pu, puk = self.pB[i % 2], ("pB", i % 2)
                sg, sgk = self.sg[i % 2], ("sg", i % 2)
                ak = [("actT", t) for t in range(nt)]
                for kc in range(KC):
                    P.mm(pg[:, 0:Tb], gv[:, kc, :], self.actT[:, kc, :], kc == 0, kc == KC - 1, r=[gk] + ak, w=[pgk])
                for kc in range(KC):
                    P.mm(pu[:, 0:Tb], uv[:, kc, :], self.actT[:, kc, :], kc == 0, kc == KC - 1, r=[uk] + ak, w=[puk])
                P.add("act", lambda e: e.activation(sg[:, 0:Tb], pg[:, 0:Tb], AF.Silu), r=[pgk], w=[sgk])
                P.add("dve", lambda e: e.tensor_tensor(self.heT[:, fc, :], sg[:, 0:Tb], pu[:, 0:Tb], ALU.mult),
                      r=[sgk, puk], w=[("heT", fc)])
            return (load, comp)

        def d_unit(e_, dc):
            def load():
                return self.wload(wd[e_, :, dc * 512:(dc + 1) * 512].rearrange("(c p) n -> p c n", p=128), (8, 512))

            def comp(wv, wk):
                for t in range(nt):
                    i = dcnt[0]
                    dcnt[0] += 1
                    pd, pdk = self.pD[i % 2], ("pD", i % 2)
                    for fc in range(8):
                        P.mm(pd[:], self.heT[:, fc, t * 128:(t + 1) * 128], wv[:, fc, :], fc == 0, fc == 7,
                             r=[wk, ("heT", fc)], w=[pdk])
                    xs = self.xres[:, t, dc * 512:(dc + 1) * 512]
                    cw = self.comb[:, t, e_:e_ + 1]
                    if e_ == 0:
                        P.add("dve", lambda e, pd=pd, xs=xs, cw=cw: e.tensor_scalar(xs, pd[:], cw, None, ALU.mult),
                              r=[pdk, ("comb", t)], w=[("xres", t)])
                    else:
                        P.add("dve", lambda e, pd=pd, xs=xs, cw=cw: e.scalar_tensor_tensor(xs, pd[:], cw, xs, ALU.mult, ALU.add),
                              r=[pdk, ("comb", t), ("xres", t)], w=[("xres", t)])
            return (load, comp)
        for e_ in range(NE):
            for fc in range(8):
                units.append(gu_unit(e_, fc))
            for dc in range(8):
                units.append(d_unit(e_, dc))
        self.pipeline(units, depth=1)
        self.load_vec_bc(1, mod[5, :])
        for t in range(nt):
            P.add("dve", lambda e, t=t: e.tensor_tensor(self.xres[:, t, :], self.xres[:, t, :], self.vb[1][:], ALU.mult),
                  r=[("xres", t), ("vb", 1)], w=[("xres", t)])
        for t in range(nt):
            P.dma("sp", self.vb[0][:], io["x1scr"][t0 + t * 128:t0 + (t + 1) * 128, :], r=[("x1scr", t0, t)], w=[("vb", 0)])
            P.add("dve", lambda e, t=t: e.scalar_tensor_tensor(self.xres[:, t, :], self.vb[0][:], ALPHA, self.xres[:, t, :],
                                                               ALU.mult, ALU.add),
                  r=[("xres", t), ("vb", 0)], w=[("xres", t)])
            self.layer_norm(t, None, None)
        self.load_vec_bc(1, io["ln"][2, :])
        for t in range(nt):
            P.add("dve", lambda e, t=t: e.tensor_tensor(self.xres[:, t, :], self.xres[:, t, :], self.vb[1][:], ALU.mult),
                  r=[("xres", t), ("vb", 1)], w=[("xres", t)])
        self.load_vec_bc(0, io["ln"][3, :])
        for t in range(nt):
            P.add("dve", lambda e, t=t: e.tensor_tensor(self.xres[:, t, :], self.xres[:, t, :], self.vb[0][:], ALU.add),
                  r=[("xres", t), ("vb", 0)], w=[("xres", t)])
            P.dma("sp", io["xout"][t0 + t * 128:t0 + (t + 1) * 128, :], self.xres[:, t, :], r=[("xres", t)])


def _mask_consts(S):
    k = np.arange(128)[:, None]
    q = np.arange(256)[None, :]
    md = np.stack([np.where(q >= k + 128 * r, 0.0, -BIG) for r in range(2)]).astype(np.float32)
    q = np.arange(512)[None, :]
    mm_ = []
    for r in range(4):
        same = (q // 256) == (r // 2)
        caus = np.where(q >= k + 128 * r, 0.0, -BIG)
        mm_.append(np.where(same, caus, 0.0))
    mm_ = np.stack(mm_).astype(np.float32)
    q = np.arange(128)[None, :]
    ms = np.stack([np.tile(np.where(q >= k, 0.0, -BIG), (1, 4)), np.tile(np.where(k > q, 0.0, -BIG), (1, 4))]).astype(np.float32)
    E = np.zeros((32, 32, 128), np.float32)
    for j in range(32):
        E[j, j, :] = 1.0
    E = E.transpose(1, 0, 2).reshape(32, 32 * 128)
    return md, mm_, ms, E


def build_attn0(S):
    C = Ctx()
    P = C.P
    NT = S // 128
    NB = S // 256
    assert NB <= 32
    qA = C.din("qA", [4, 128, S], BF16)
    kA = C.din("kA", [4, 128, S], BF16)
    vA = C.din("vA", [S, 512], BF16)
    qB = C.din("qB", [4, 128, S], BF16)
    kB = C.din("kB", [4, 128, S], BF16)
    vB = C.din("vB", [S, 512], BF16)
    lamv = C.din("lamv", [4, 128], F32)
    subg = C.din("subg", [1, 256], F32)
    ident_d = C.din("ident", [128, 128], F32)
    md_d = C.din("md", [2, 128, 256], F32)
    mm_d = C.din("mm", [4, 128, 512], F32)
    E_d = C.din("E", [32, 32 * 128], F32)
    o_out = C.dout("o", [S, 1024], BF16)
    identf = C.sb("identf", [128, 128], F32)
    identb = C.sb("identb", [128, 128], BF16)
    mdb = C.sb("mdb", [128, 2, 256], BF16)
    mmb = C.sb("mmb", [128, 4, 512], BF16)
    Eb = C.sb("Eb", [32, 32 * 128], BF16)
    lam_s = C.sb("lam_s", [128, 4, 128], F32)
    lamt = C.sb("lamt", [128, 8], F32)
    gsub = C.sb("gsub", [128, 256], F32)
    QA = [C.sb("QA%d" % m, [128, S], BF16) for m in range(2)]
    KA = [C.sb("KA%d" % m, [128, S], BF16) for m in range(2)]
    VA = C.sb("VA", [128, NT, 257], BF16)
    QB = C.sb("QB", [128, S], BF16)
    KB = C.sb("KB", [128, S], BF16)
    VB = C.sb("VB", [128, NT, 129], BF16)
    negT = C.sb("negT", [32, S], BF16)
    kmean = C.sb("kmean", [128, 32], F32)
    kmeanb = C.sb("kmeanb", [128, 32], BF16)
    gate = C.sb("gate", [128, 32], F32)
    negm = C.sb("negm", [128, 32], F32)
    top8 = C.sb("top8", [128, 8], F32)
    PT = [C.sb("PT%d" % i, [128, 512], BF16) for i in range(2)]
    fin = {nm: C.sb("fin_" + nm, [128, 4], F32) for nm in ("r1", "r2", "ss", "rs")}
    ot = [C.sb("ot%d" % i, [128, 256], F32) for i in range(2)]
    ou = [C.sb("ou%d" % i, [128, 256], F32) for i in range(2)]
    junk = C.sb("junk", [128, 256], BF16)
    ost = [C.sb("ost%d" % i, [128, 256], BF16) for i in range(2)]
    stage = C.sb("stage", [128, 512], F32)
    pS = [C.ps("pS%d" % i, [128, 512]) for i in range(2)]
    pO = [C.ps("pO%d" % i, [128, 512]) for i in range(4)]
    pG = C.ps("pG", [128, 512])
    pX = C.ps("pX", [128, 512])
    P.dma("sp", identf[:], ident_d, w=["identf"])
    P.add("dve", lambda e: e.tensor_copy(identb[:], identf[:]), r=["identf"], w=["identb"])
    for r_ in range(2):
        P.dma("pool", mdb[:, r_, :], md_d[r_], w=["mdb"])
    for r_ in range(4):
        P.dma("pool", mmb[:, r_, :], mm_d[r_], w=["mmb"])
    P.dma("pool", Eb[:], E_d, w=["Eb"])
    for i in range(4):
        P.dma("sp", lam_s[:, i, :], lamv[i, :].partition_broadcast(128), w=[("lam_s", i)])
    P.dma("sp", gsub[:], subg[0, :].partition_broadcast(128), w=["gsub"])
    P.add("dve", lambda e: e.tensor_tensor(lam_s[:, 0, :], lam_s[:, 0, :], lam_s[:, 1, :], ALU.mult),
          r=[("lam_s", 0), ("lam_s", 1)], w=[("lam_s", 0)])
    P.add("dve", lambda e: e.tensor_tensor(lam_s[:, 2, :], lam_s[:, 2, :], lam_s[:, 3, :], ALU.mult),
          r=[("lam_s", 2), ("lam_s", 3)], w=[("lam_s", 2)])
    P.add("dve", lambda e: e.tensor_reduce(lamt[:, 0:1], lam_s[:, 0, :], AX.X, ALU.add), r=[("lam_s", 0)], w=["lamt"])
    P.add("dve", lambda e: e.tensor_reduce(lamt[:, 1:2], lam_s[:, 2, :], AX.X, ALU.add), r=[("lam_s", 2), "lamt"], w=["lamt"])
    P.add("act", lambda e: e.activation(lamt[:, 2:4], lamt[:, 0:2], AF.Exp), r=["lamt"], w=["lamt2"])
    P.add("dve", lambda e: e.tensor_tensor(lamt[:, 4:5], lamt[:, 2:3], lamt[:, 3:4], ALU.subtract), r=["lamt2"], w=["lamt3"])
    P.add("dve", lambda e: e.tensor_scalar(lamt[:, 5:6], lamt[:, 4:5], LAM_INIT, -1.0, ALU.add, ALU.mult),
          r=["lamt3"], w=["neglam"])
    P.add("dve", lambda e: e.tensor_scalar_mul(gsub[:], gsub[:], 1.0 - LAM_INIT), r=["gsub"], w=["gsub"])
    nstep = [0]

    for h in range(2):
        for m in range(2):
            P.dma("sp", QA[m][:], qA[2 * h + m], w=[("QA", m)])
            P.dma("sp", KA[m][:], kA[2 * h + m], w=[("KA", m)])
        P.add("pool", lambda e: e.memset(VA[:, :, 256:257], 1.0), w=["VA"])
        P.dma("sp", VA[:, :, 0:256], vA[:, h * 256:(h + 1) * 256].rearrange("(t p) n -> p t n", p=128), w=["VA"])
        for qc in range(S // 256):
            nk = 2 * qc + 2
            for kt in range(nk):
                i = nstep[0]
                nstep[0] += 1
                ps, psk = pS[i % 2], ("pS", i % 2)
                pt, ptk = PT[i % 2], ("PT", i % 2)
                diag = kt >= 2 * qc
                for m in range(2):
                    P.mm(ps[:, m * 256:(m + 1) * 256], KA[m][:, kt * 128:(kt + 1) * 128], QA[m][:, qc * 256:(qc + 1) * 256],
                         True, not diag, r=[("KA", m), ("QA", m)], w=[psk])
                    if diag:
                        P.mm(ps[:, m * 256:(m + 1) * 256], identb[:], mdb[:, kt - 2 * qc, :], False, True,
                             r=["identb", "mdb"], w=[psk])
                P.add("act", lambda e, ps=ps, pt=pt: e.activation(pt[:], ps[:], AF.Exp, scale=SCALE), r=[psk], w=[ptk])
                for m in range(2):
                    for t in range(2):
                        if kt <= 2 * qc + t:
                            P.mm(pO[m * 2 + t][:, 0:257], pt[:, m * 256 + t * 128:m * 256 + (t + 1) * 128], VA[:, kt, :],
                                 kt == 0, kt == 2 * qc + t, r=[ptk, "VA"], w=[("pO", m * 2 + t)])
            for t in range(2):
                j = t
                o1, o2 = pO[t], pO[2 + t]
                k1, k2 = ("pO", t), ("pO", 2 + t)
                fk = ("fin", j)
                P.add("dve", lambda e, o1=o1, j=j: e.reciprocal(fin["r1"][:, j:j + 1], o1[:, 256:257]), r=[k1], w=[fk + ("r1",)])
                P.add("dve", lambda e, o2=o2, j=j: e.reciprocal(fin["r2"][:, j:j + 1], o2[:, 256:257]), r=[k2], w=[fk + ("r2",)])
                P.add("dve", lambda e, j=j: e.tensor_tensor(fin["r2"][:, j:j + 1], fin["r2"][:, j:j + 1], lamt[:, 5:6], ALU.mult),
                      r=[fk + ("r2",), "neglam"], w=[fk + ("r2",)])
                P.add("dve", lambda e, o1=o1, j=j: e.tensor_scalar(ot[j][:], o1[:, 0:256], fin["r1"][:, j:j + 1], None, ALU.mult),
                      r=[k1, fk + ("r1",)], w=[("ot", j)])
                P.add("dve", lambda e, o2=o2, j=j: e.scalar_tensor_tensor(ou[j][:], o2[:, 0:256], fin["r2"][:, j:j + 1], ot[j][:],
                                                                          ALU.mult, ALU.add),
                      r=[k2, fk + ("r2",), ("ot", j)], w=[("ou", j)])
                P.add("dve", lambda e, j=j: e.memset(fin["ss"][:, j:j + 1], 0.0), w=[fk + ("ss",)])
                P.add("act", lambda e, j=j: e.activation(junk[:], ou[j][:], AF.Square, accum_out=fin["ss"][:, j:j + 1]),
                      r=[("ou", j), fk + ("ss",)], w=["junk", fk + ("ss",)])
                P.add("dve", lambda e, j=j: e.tensor_scalar(fin["rs"][:, j:j + 1], fin["ss"][:, j:j + 1], 1.0 / 256.0, RMS_EPS,
                                                            ALU.mult, ALU.add), r=[fk + ("ss",)], w=[fk + ("rs",)])
                P.add("dve", lambda e, j=j: e.reciprocal(fin["rs"][:, j:j + 1], fin["rs"][:, j:j + 1]),
                      r=[fk + ("rs",)], w=[fk + ("rs",)])
                P.add("act", lambda e, j=j: e.activation(fin["rs"][:, j:j + 1], fin["rs"][:, j:j + 1], AF.Sqrt),
                      r=[fk + ("rs",)], w=[fk + ("rs",)])
                P.add("dve", lambda e, j=j: e.scalar_tensor_tensor(ost[j][:], ou[j][:], fin["rs"][:, j:j + 1], gsub[:],
                                                                   ALU.mult, ALU.mult),
                      r=[("ou", j), fk + ("rs",), "gsub"], w=[("ost", j)])
                tq = 2 * qc + t
                P.dma("sp", o_out[tq * 128:(tq + 1) * 128, h * 256:(h + 1) * 256], ost[j][:], r=[("ost", j)])

    for h in range(4):
        P.dma("sp", QB[:], qB[h], w=["QB"])
        P.dma("sp", KB[:], kB[h], w=["KB"])
        P.add("pool", lambda e: e.memset(VB[:, :, 128:129], 1.0), w=["VB"])
        P.dma("sp", VB[:, :, 0:128], vB[:, h * 128:(h + 1) * 128].rearrange("(t p) n -> p t n", p=128), w=["VB"])
        P.add("dve", lambda e: e.tensor_reduce(kmean[:, 0:NB], KB[:].rearrange("p (n k) -> p n k", k=256), AX.X, ALU.add),
              r=["KB"], w=["kmean"])
        P.add("dve", lambda e: e.tensor_scalar_mul(kmeanb[:, 0:NB], kmean[:, 0:NB], 1.0 / 256.0), r=["kmean"], w=["kmeanb"])
        for t in range(NT):
            own = t // 2
            P.add("dve", lambda e: e.memset(negm[:], -BIG), w=["negm"])
            if own >= 1:
                if own > 3:
                    P.mm(pG[:, 0:NB], QB[:, t * 128:(t + 1) * 128], kmeanb[:, 0:NB], True, True, r=["QB", "kmeanb"], w=["pG"])
                    P.add("dve", lambda e: e.memset(gate[:], -BIG), w=["gate"])
                    P.add("dve", lambda e, own=own: e.tensor_copy(gate[:, 0:own], pG[:, 0:own]), r=["pG", "gate"], w=["gate"])
                    P.add("dve", lambda e: e.max(top8[:], gate[:]), r=["gate"], w=["top8"])
                    P.add("dve", lambda e, own=own: e.tensor_scalar(negm[:, 0:own], gate[:, 0:own], top8[:, 2:3], None, ALU.is_ge),
                          r=["gate", "top8", "negm"], w=["negm"])
                    P.add("dve", lambda e, own=own: e.tensor_scalar(negm[:, 0:own], negm[:, 0:own], 1.0, BIG, ALU.subtract, ALU.mult),
                          r=["negm"], w=["negm"])
                else:
                    P.add("dve", lambda e, own=own: e.memset(negm[:, 0:own], 0.0), r=["negm"], w=["negm"])
            P.add("dve", lambda e, own=own: e.memset(negm[:, own:own + 1], 0.0), r=["negm"], w=["negm"])
            P.tr(pX[0:32, 0:128], negm[:], identf[:], r=["negm", "identf"], w=["pX"])
            P.add("act", lambda e, t=t: e.activation(negT[:, t * 128:(t + 1) * 128], pX[0:32, 0:128], AF.Identity),
                  r=["pX"], w=["negT"])
        for c in range(S // 512):
            nk = 4 * c + 4
            for kt in range(nk):
                i = nstep[0]
                nstep[0] += 1
                ps, psk = pS[i % 2], ("pS", i % 2)
                pt, ptk = PT[i % 2], ("PT", i % 2)
                j = kt // 2
                P.mm(ps[:], KB[:, kt * 128:(kt + 1) * 128], QB[:, c * 512:(c + 1) * 512], True, False, r=["KB", "QB"], w=[psk])
                last_sel = kt < 4 * c
                P.mm(ps[:], Eb[:, j * 128:(j + 1) * 128], negT[:, c * 512:(c + 1) * 512], False, last_sel,
                     r=["Eb", "negT"], w=[psk])
                if not last_sel:
                    P.mm(ps[:], identb[:], mmb[:, kt - 4 * c, :], False, True, r=["identb", "mmb"], w=[psk])
                P.add("act", lambda e, ps=ps, pt=pt: e.activation(pt[:], ps[:], AF.Exp, scale=SCALE), r=[psk], w=[ptk])
                for tq in range(4):
                    if kt <= 4 * c + tq:
                        P.mm(pO[tq][:, 0:129], pt[:, tq * 128:(tq + 1) * 128], VB[:, kt, :], kt == 0, kt == 4 * c + tq,
                             r=[ptk, "VB"], w=[("pO", tq)])
            for tq in range(4):
                j = tq % 2
                fk = ("fin", j)
                P.add("dve", lambda e, tq=tq, j=j: e.reciprocal(fin["r1"][:, j:j + 1], pO[tq][:, 128:129]),
                      r=[("pO", tq)], w=[fk + ("r1",)])
                P.add("dve", lambda e, tq=tq, j=j: e.tensor_scalar(ost[j][:, 0:128], pO[tq][:, 0:128], fin["r1"][:, j:j + 1], None, ALU.mult),
                      r=[("pO", tq), fk + ("r1",)], w=[("ost", j)])
                tt = 4 * c + tq
                P.dma("sp", o_out[tt * 128:(tt + 1) * 128, 512 + h * 128:512 + (h + 1) * 128], ost[j][:, 0:128], r=[("ost", j)])
    return C.finish()


def build_attn1(S):
    C = Ctx()
    P = C.P
    NT = S // 128
    q_d = C.din("q", [8, 128, S], BF16)
    k_d = C.din("k", [2, 128, S], BF16)
    v_d = C.din("v", [S, 256], BF16)
    sk_d = C.din("sinks", [1, 8], F32)
    ident_d = C.din("ident", [128, 128], F32)
    ms_d = C.din("ms", [2, 128, 512], F32)
    o_out = C.dout("o", [S, 1024], BF16)
    identf = C.sb("identf", [128, 128], F32)
    identb = C.sb("identb", [128, 128], BF16)
    msb = C.sb("msb", [128, 2, 512], BF16)
    esink = C.sb("esink", [128, 8], F32)
    Q = C.sb("Q", [128, 4, S], BF16)
    K = C.sb("K", [128, S], BF16)
    V = C.sb("V", [128, NT, 129], BF16)
    PT = [C.sb("PT%d" % i, [128, 512], BF16) for i in range(2)]
    den = C.sb("den", [128, 8], F32)
    ost = [C.sb("ost%d" % i, [128, 512], BF16) for i in range(2)]
    pS = [C.ps("pS%d" % i, [128, 512]) for i in range(2)]
    pO = [C.ps("pO%d" % i, [128, 512]) for i in range(4)]
    P.dma("sp", identf[:], ident_d, w=["identf"])
    P.add("dve", lambda e: e.tensor_copy(identb[:], identf[:]), r=["identf"], w=["identb"])
    for r_ in range(2):
        P.dma("pool", msb[:, r_, :], ms_d[r_], w=["msb"])
    P.dma("sp", esink[:], sk_d[0, :].partition_broadcast(128), w=["esink"])
    P.add("act", lambda e: e.activation(esink[:], esink[:], AF.Exp), r=["esink"], w=["esink"])
    n = 0
    for j in range(2):
        for hh in range(4):
            P.dma("sp", Q[:, hh, :], q_d[4 * j + hh], w=["Q"])
        P.dma("sp", K[:], k_d[j], w=["K"])
        P.add("pool", lambda e: e.memset(V[:, :, 128:129], 1.0), w=["V"])
        P.dma("sp", V[:, :, 0:128], v_d[:, j * 128:(j + 1) * 128].rearrange("(t p) n -> p t n", p=128), w=["V"])
        for t in range(NT):
            kts = [t - 1, t] if t > 0 else [t]
            for kt in kts:
                ps, psk = pS[n % 2], ("pS", n % 2)
                pt, ptk = PT[n % 2], ("PT", n % 2)
                n += 1
                P.mm(ps[:].rearrange("p (h q) -> p h q", q=128), K[:, kt * 128:(kt + 1) * 128], Q[:, :, t * 128:(t + 1) * 128],
                     True, False, r=["K", "Q"], w=[psk])
                P.mm(ps[:], identb[:], msb[:, 0 if kt == t else 1, :], False, True, r=["identb", "msb"], w=[psk])
                P.add("act", lambda e, ps=ps, pt=pt: e.activation(pt[:], ps[:], AF.Exp, scale=SCALE), r=[psk], w=[ptk])
                for hh in range(4):
                    P.mm(pO[hh][:, 0:129], pt[:, hh * 128:(hh + 1) * 128], V[:, kt, :], kt == kts[0], kt == t,
                         r=[ptk, "V"], w=[("pO", hh)])
            os_, osk = ost[t % 2], ("ost", t % 2)
            for hh in range(4):
                hq = 4 * j + hh
                P.add("dve", lambda e, hh=hh, hq=hq: e.tensor_tensor(den[:, hh:hh + 1], pO[hh][:, 128:129], esink[:, hq:hq + 1], ALU.add),
                      r=[("pO", hh), "esink"], w=[("den", hh)])
                P.add("dve", lambda e, hh=hh: e.reciprocal(den[:, 4 + hh:5 + hh], den[:, hh:hh + 1]), r=[("den", hh)], w=[("den", hh)])
                P.add("dve", lambda e, hh=hh, os_=os_: e.tensor_scalar(os_[:, hh * 128:(hh + 1) * 128], pO[hh][:, 0:128],
                                                                        den[:, 4 + hh:5 + hh], None, ALU.mult),
                      r=[("pO", hh), ("den", hh)], w=[osk])
            P.dma("sp", o_out[t * 128:(t + 1) * 128, j * 512:(j + 1) * 512], os_[:], r=[osk])
    return C.finish()


_CACHE = {}
_DBG = {}


def _run(nc, in_maps):
    res = run_bass_kernel_spmd(nc, in_maps, core_ids=list(range(8)))
    return res.results


def kernel(x, c, positions, router_w, router_bias,
           l0_w_ada, l0_b_ada, l0_w_in, l0_lambda_q1, l0_lambda_k1, l0_lambda_q2, l0_lambda_k2,
           l0_subln_g, l0_w_out, l0_ln1_g, l0_ln1_b, l0_w_gate, l0_w_up, l0_w_down, l0_ln2_g, l0_ln2_b,
           l1_w_ada, l1_b_ada, l1_w_in, l1_b_in, l1_sinks, l1_w_out, l1_ln1_g, l1_ln1_b,
           l1_w_gate, l1_w_up, l1_w_down, l1_ln2_g, l1_ln2_b):
    f32 = lambda a: np.ascontiguousarray(np.asarray(a, dtype=np.float32))
    x = f32(x)
    B, S, _ = x.shape
    Tc = S // 4
    ident, r32t, invf = _consts()
    md, mmk, ms, E = _mask_consts(S)
    pos = np.ascontiguousarray(np.asarray(positions, dtype=np.int32))
    cores = [(b, g) for b in range(2) for g in range(4)]

    ncol = 6 * D // 8
    nc = build_ada(ncol)
    c32 = f32(c)
    w0, w1 = f32(l0_w_ada), f32(l1_w_ada)
    b0, b1 = f32(l0_b_ada), f32(l1_b_ada)
    maps = [dict(c=c32, w0=np.ascontiguousarray(w0[:, i * ncol:(i + 1) * ncol]), b0=b0[None, i * ncol:(i + 1) * ncol],
                 w1=np.ascontiguousarray(w1[:, i * ncol:(i + 1) * ncol]), b1=b1[None, i * ncol:(i + 1) * ncol]) for i in range(8)]
    r = _run(nc, maps)
    m0 = np.concatenate([r[i]["m0"] for i in range(8)], axis=1)
    m1 = np.concatenate([r[i]["m1"] for i in range(8)], axis=1)
    mod0 = [np.ascontiguousarray(m0[b].reshape(6, D)) for b in range(2)]
    mod1 = [np.ascontiguousarray(m1[b].reshape(6, D)) for b in range(2)]
    del w0, w1, maps
    _DBG['m0'] = m0
    _DBG['m1'] = m1

    xs = [np.ascontiguousarray(x[b, g * Tc:(g + 1) * Tc]) for b, g in cores]
    ps_ = [np.ascontiguousarray(pos[b, g * Tc:(g + 1) * Tc][None, :]) for b, g in cores]
    zb0 = np.zeros((1, 12288), np.float32)

    st = TokStage(Tc, [("front", 0)])
    w_in0 = f32(l0_w_in)
    maps = [dict(ident=ident, xin=xs[i], mod0=mod0[cores[i][0]], w_in=w_in0, pos=ps_[i], r32t=r32t, invf=invf)
            for i in range(8)]
    r = _run(st.nc, maps)
    qk = [[r[b * 4 + g]["qkT"] for g in range(4)] for b in range(2)]
    vv = [[r[b * 4 + g]["vout"] for g in range(4)] for b in range(2)]
    del maps
    _DBG['qk0'] = qk
    _DBG['v0'] = vv

    nc = build_attn0(S)
    lamv = np.stack([f32(l0_lambda_q1), f32(l0_lambda_k1), f32(l0_lambda_q2), f32(l0_lambda_k2)])
    subg = f32(l0_subln_g)[None, :]
    maps = []
    for b, g in cores:
        full = np.concatenate(qk[b], axis=2)
        vfull = np.concatenate(vv[b], axis=0)
        maps.append(dict(
            qA=np.ascontiguousarray(full[0 + 4 * g:0 + 4 * g + 4]), kA=np.ascontiguousarray(full[16 + 4 * g:16 + 4 * g + 4]),
            qB=np.ascontiguousarray(full[32 + 4 * g:32 + 4 * g + 4]), kB=np.ascontiguousarray(full[48 + 4 * g:48 + 4 * g + 4]),
            vA=np.ascontiguousarray(vfull[:, 512 * g:512 * (g + 1)]), vB=np.ascontiguousarray(vfull[:, 2048 + 512 * g:2048 + 512 * (g + 1)]),
            lamv=lamv, subg=subg, ident=ident, md=md, mm=mmk, E=E))
    r = _run(nc, maps)
    o0 = []
    for b in range(2):
        oa = np.concatenate([r[b * 4 + g]["o"][:, 0:512] for g in range(4)], axis=1)
        ob = np.concatenate([r[b * 4 + g]["o"][:, 512:1024] for g in range(4)], axis=1)
        o0.append(np.concatenate([oa, ob], axis=1))
    del maps, qk, vv
    _DBG['o0'] = o0

    st = TokStage(Tc, [("back", 0), ("front", 1)])
    ln0 = np.stack([f32(l0_ln1_g), f32(l0_ln1_b), f32(l0_ln2_g), f32(l0_ln2_b)])
    rw, rbias = f32(router_w), f32(router_bias)[None, :]
    w_in1, b_in1 = f32(l1_w_in), f32(l1_b_in)[None, :]
    wo0, wg0, wu0, wd0 = f32(l0_w_out), f32(l0_w_gate), f32(l0_w_up), f32(l0_w_down)
    maps = [dict(ident=ident, xin=xs[i], mod0=mod0[cores[i][0]], mod1=mod1[cores[i][0]],
                 oin=np.ascontiguousarray(o0[cores[i][0]][cores[i][1] * Tc:(cores[i][1] + 1) * Tc]),
                 w_out=wo0, ln=ln0, router_w=rw, router_b=rbias, w_gate=wg0, w_up=wu0, w_down=wd0,
                 w_in=w_in1, b_in=b_in1, pos=ps_[i], r32t=r32t, invf=invf) for i in range(8)]
    r = _run(st.nc, maps)
    x1 = [r[i]["xout"] for i in range(8)]
    qk = [[r[b * 4 + g]["qkT"] for g in range(4)] for b in range(2)]
    vv = [[r[b * 4 + g]["vout"] for g in range(4)] for b in range(2)]
    del maps, wo0, wg0, wu0, wd0
    _DBG['x1'] = x1
    _DBG['qk1'] = qk
    _DBG['v1'] = vv

    nc = build_attn1(S)
    sinks = f32(l1_sinks)
    maps = []
    for b, g in cores:
        full = np.concatenate(qk[b], axis=2)
        vfull = np.concatenate(vv[b], axis=0)
        maps.append(dict(q=np.ascontiguousarray(full[8 * g:8 * g + 8]), k=np.ascontiguousarray(full[32 + 2 * g:32 + 2 * g + 2]),
                         v=np.ascontiguousarray(vfull[:, 256 * g:256 * (g + 1)]), sinks=sinks[None, 8 * g:8 * g + 8],
                         ident=ident, ms=ms))
    r = _run(nc, maps)
    o1 = [np.concatenate([r[b * 4 + g]["o"] for g in range(4)], axis=1) for b in range(2)]
    del maps, qk, vv
    _DBG['o1'] = o1

    st = TokStage(Tc, [("back", 1)])
    ln1 = np.stack([f32(l1_ln1_g), f32(l1_ln1_b), f32(l1_ln2_g), f32(l1_ln2_b)])
    wo1, wg1, wu1, wd1 = f32(l1_w_out), f32(l1_w_gate), f32(l1_w_up), f32(l1_w_down)
    maps = [dict(ident=ident, xin=x1[i], mod1=mod1[cores[i][0]],
                 oin=np.ascontiguousarray(o1[cores[i][0]][cores[i][1] * Tc:(cores[i][1] + 1) * Tc]),
                 w_out=wo1, ln=ln1, router_w=rw, router_b=rbias, w_gate=wg1, w_up=wu1, w_down=wd1) for i in range(8)]
    r = _run(st.nc, maps)
    out = np.empty((B, S, D), np.float32)
    for i, (b, g) in enumerate(cores):
        out[b, g * Tc:(g + 1) * Tc] = r[i]["xout"]
    return out
```

```python
import contextlib
import math
import numpy as np
import concourse.bass as bass
import concourse.mybir as mybir
from concourse.bass_utils import run_bass_kernel_spmd

F32 = mybir.dt.float32
BF16 = mybir.dt.bfloat16
I32 = mybir.dt.int32
ALU = mybir.AluOpType
AF = mybir.ActivationFunctionType
AX = mybir.AxisListType

D = 4096
KC = D // 128
HD = 128
NE = 16
DFF = 1024
BIG = 30000.0
ALPHA = float(4 ** 0.25)
LN_EPS = 1e-5
RMS_EPS = 1e-5
LAM_INIT = 0.8 - 0.6 * math.exp(0.0)
SCALE = HD ** -0.5
PI = math.pi

ENGS = ("pe", "act", "dve", "pool", "sp")


class Op:
    __slots__ = ("eng", "fn", "deps", "marked", "cnt", "is_dma", "dsem", "dcnt", "prev_dcnt")

    def __init__(self, eng, fn, is_dma):
        self.eng = eng
        self.fn = fn
        self.deps = ()
        self.marked = False
        self.cnt = 0
        self.is_dma = is_dma
        self.dsem = -1
        self.dcnt = 0
        self.prev_dcnt = 0


class Prog:
    def __init__(self, nc, n_dma_sems=32):
        self.nc = nc
        self.q = {e: [] for e in ENGS}
        self.st = {}
        self.n_dma_sems = n_dma_sems
        self.dma_order = []
        self.last_compute = {}
        self.pending = {}
        self.ncc = 0
        self.ccsem = None
        self.ccscratch = None

    def add(self, eng, fn, r=(), w=(), dma=False):
        op = Op(eng, fn, dma)
        st = self.st
        deps = set()
        for k in r:
            s = st.get(k)
            if s is not None and s[0] is not None:
                deps.add(s[0])
        for k in w:
            s = st.get(k)
            if s is not None:
                if s[0] is not None:
                    deps.add(s[0])
                for x in s[1]:
                    deps.add(x)
        for k in r:
            s = st.get(k)
            if s is None:
                st[k] = [None, [op]]
            else:
                if not dma:
                    s[1][:] = [x for x in s[1] if x.is_dma or x.eng != eng]
                s[1].append(op)
        for k in w:
            st[k] = [op, []]
        deps.discard(op)
        if eng in self.pending:
            deps |= set(self.pending.pop(eng))
        if not dma:
            self.last_compute[eng] = op
        if not dma and eng == "pe":
            deps = [d for d in deps if d.is_dma or d.eng != "pe"]
        op.deps = tuple(deps)
        self.q[eng].append(op)
        if dma:
            self.dma_order.append(op)
        return op

    def barrier(self):
        lastd = list(self.dma_order[-self.n_dma_sems:])
        for e in ENGS:
            d = [op for e2, op in self.last_compute.items() if e2 != e]
            self.pending[e] = list(self.pending.get(e, [])) + d + lastd

    def cc(self, kind, ins, outs, groups):
        self.barrier()
        self.ncc += 1
        k = self.ncc

        def fn(e):
            e.collective_compute(kind, ALU.bypass, replica_groups=groups, ins=[ins], outs=[outs]).then_inc(self.ccsem, 1)
            e.wait_ge(self.ccsem, k)
            return e.memset(self.ccscratch[:], 0.0)
        self.add("pool", fn)
        self.barrier()

    def gather(self, out, in_, idx, r=(), w=()):
        return self.add("pool", lambda e: e.indirect_dma_start(
            out=out, out_offset=None, in_=in_, in_offset=bass.IndirectOffsetOnAxis(ap=idx, axis=0)), r, w, dma=True)

    def dma(self, eng, out, in_, r=(), w=(), **kw):
        return self.add(eng, lambda e: e.dma_start(out=out, in_=in_, **kw), r, w, dma=True)

    def mm(self, out, lhsT, rhs, start, stop, r=(), w=()):
        return self.add("pe", lambda e: e.matmul(out, lhsT, rhs, start=start, stop=stop), r, w)

    def tr(self, out, in_, ident, r=(), w=()):
        return self.add("pe", lambda e: e.transpose(out, in_, ident), r, w)

    def emit(self):
        nc = self.nc
        for e in ENGS:
            for op in self.q[e]:
                for d in op.deps:
                    d.marked = True
        for e in ENGS:
            c = 0
            for op in self.q[e]:
                if not op.is_dma and op.marked:
                    c += 1
                op.cnt = c
        uses = [0] * self.n_dma_sems
        for k, op in enumerate(self.dma_order):
            s = k % self.n_dma_sems
            op.dsem = s
            op.prev_dcnt = 16 * uses[s]
            uses[s] += 1
            op.dcnt = 16 * uses[s]
        final_dma = [16 * u for u in uses]

        with contextlib.ExitStack() as es:
            esem = {e: es.enter_context(nc.semaphore("s_" + e)) for e in ENGS}
            dsems = [es.enter_context(nc.semaphore("d_%d" % i)) for i in range(self.n_dma_sems)]
            self.ccsem = es.enter_context(nc.semaphore("ccsem"))
            block = es.enter_context(nc.Block())

            def run(e, eng):
                seen = {}
                for op in self.q[e]:
                    for d in op.deps:
                        if d.is_dma:
                            key, thr, sem = ("d", d.dsem), d.dcnt, dsems[d.dsem]
                        else:
                            key, thr, sem = ("e", d.eng), d.cnt, esem[d.eng]
                        if seen.get(key, 0) < thr:
                            eng.wait_ge(sem, thr)
                            seen[key] = thr
                    if op.is_dma:
                        key = ("d", op.dsem)
                        if op.prev_dcnt > 0 and seen.get(key, 0) < op.prev_dcnt:
                            eng.wait_ge(dsems[op.dsem], op.prev_dcnt)
                            seen[key] = op.prev_dcnt
                        op.fn(eng).then_inc(dsems[op.dsem], 16)
                    else:
                        ins = op.fn(eng)
                        if op.marked:
                            ins.then_inc(esem[e], 1)
                if e == "sp":
                    for i, v in enumerate(final_dma):
                        if v > 0:
                            eng.wait_ge(dsems[i], v)

            block.tensor(lambda eng: run("pe", eng))
            block.scalar(lambda eng: run("act", eng))
            block.vector(lambda eng: run("dve", eng))
            block.gpsimd(lambda eng: run("pool", eng))
            block.sync(lambda eng: run("sp", eng))


class Ctx:
    ARENA = 52736

    def __init__(self):
        self.nc = bass.Bass("TRN2", target_bir_lowering=False)
        self.es = contextlib.ExitStack()
        self.P = Prog(self.nc, n_dma_sems=24)
        self.arena = self.es.enter_context(self.nc.sbuf_tensor("arena", [128, self.ARENA], F32))
        self.P.ccscratch = self.es.enter_context(self.nc.sbuf_tensor("ccscr", [1, 8], F32))
        self.banks = [self.es.enter_context(self.nc.psum_tensor("bank%d" % i, [128, 512], F32)) for i in range(8)]
        self.off = 0
        self.nbank = 0
        self.stage = 0

    def new_stage(self):
        self.P.barrier()
        self.off = 0
        self.nbank = 0
        self.stage += 1

    def key(self, k):
        return (self.stage, k)

    def din(self, name, shape, dt):
        return self.nc.dram_tensor(name, list(shape), dt, kind="ExternalInput").ap()

    def dout(self, name, shape, dt):
        return self.nc.dram_tensor(name, list(shape), dt, kind="ExternalOutput").ap()

    def dscr(self, name, shape, dt):
        return self.nc.dram_tensor(name, list(shape), dt, kind="Internal").ap()

    def sb(self, name, shape, dt):
        esz = 2 if dt == BF16 else 4
        n = 1
        for d in shape[1:]:
            n *= d
        words = (n * esz + 3) // 4
        words = (words + 7) // 8 * 8
        ap = self.arena[0:shape[0], self.off:self.off + words]
        self.off += words
        assert self.off <= self.ARENA, (name, self.off)
        if dt != F32:
            ap = ap.bitcast(dt)
        if esz == 2 and 2 * words != n:
            ap = ap[:, 0:n]
        if esz == 4 and words != n:
            ap = ap[:, 0:n]
        if len(shape) == 3:
            ap = ap.rearrange("p (a b) -> p a b", b=shape[2])
        return ap

    def ps(self, name, shape, dt=F32):
        b = self.banks[self.nbank]
        self.nbank += 1
        n = 1
        for d in shape[1:]:
            n *= d
        ap = b[0:shape[0], 0:n]
        if len(shape) == 3:
            ap = ap.rearrange("p (a b) -> p a b", b=shape[2])
        return ap

    def finish(self):
        self.P.emit()
        self.es.close()
        return self.nc


def _consts():
    ident = np.eye(128, dtype=np.float32)
    r32t = np.zeros((32, 32), np.float32)
    for i in range(16):
        r32t[i + 16, i] = -1.0
        r32t[i, i + 16] = 1.0
    inv = (500000.0 ** (-np.arange(0, 32, 2, dtype=np.float32) / 32.0)).astype(np.float32)
    invf = np.concatenate([inv, inv]).reshape(32, 1).astype(np.float32)
    return ident, r32t, invf


GROUPS = [[0, 1, 2, 3], [4, 5, 6, 7]]


def stage_ada(C, io):
    P = C.P
    NCOL = 3072
    cT = C.sb("cT", [128, KC], F32)
    sT = C.sb("sT", [128, KC], F32)
    wk = [C.sb("wk%d" % i, [128, NCOL], F32) for i in range(3)]
    bb = C.sb("bb", [1, NCOL], F32)
    res = C.sb("res", [1, NCOL], F32)
    pm = [C.ps("pm%d" % i, [1, 512]) for i in range(6)]
    P.dma("sp", cT[:], io["c_own"][0, :].rearrange("(c p) -> p c", p=128), w=["cT"], allow_slow_non_contiguous=True)
    P.add("act", lambda e: e.activation(sT[:], cT[:], AF.Silu), r=["cT"], w=["sT"])
    n = 0
    for li in range(2):
        w = io["wada%d" % li]
        b = io["bada%d" % li]
        for half in range(2):
            c0 = half * NCOL
            for kc in range(KC):
                s_ = n % 3
                n += 1
                P.dma("sp", wk[s_][:], w[kc * 128:(kc + 1) * 128, c0:c0 + NCOL], w=[("wk", s_)])
                for j in range(6):
                    P.mm(pm[j][:], sT[:, kc:kc + 1], wk[s_][:, j * 512:(j + 1) * 512], kc == 0, kc == KC - 1,
                         r=["sT", ("wk", s_)], w=[("pm", j)])
            P.dma("sp", bb[:], b[0:1, c0:c0 + NCOL], w=["bb"])
            for j in range(6):
                P.add("dve", lambda e, j=j: e.tensor_tensor(res[:, j * 512:(j + 1) * 512], pm[j][:],
                                                            bb[:, j * 512:(j + 1) * 512], ALU.add),
                      r=[("pm", j), "bb"], w=[("res", j)])
            P.dma("sp", io["ada_in"][li:li + 1, c0:c0 + NCOL], res[:], r=[("res", j) for j in range(6)])
    P.cc("AllGather", io["ada_in"], io["ada_g"], GROUPS)
    for li in range(2):
        src = io["ada_g"].rearrange("(r l) (k j) -> l k r j", l=2, k=6)
        P.dma("sp", io["mod"][li].rearrange("k (r j) -> k r j", r=4), src[li], allow_slow_non_contiguous=True)


class TokStage:
    def __init__(self, C, Tc, parts, io):
        self.Tc = Tc
        self.Tb = min(512, Tc)
        self.nt = self.Tb // 128
        self.nblk = Tc // self.Tb
        self.parts = parts
        self.C = C
        self.io = io
        self.build()

    def wload(self, src_ap, shape3):
        s = self.wnext % self.nslots
        self.wnext += 1
        a, b = shape3
        view = self.wb[s][:, 0:a * b].rearrange("p (a b) -> p a b", b=b)
        self.C.P.dma("pool", view, src_ap, w=[("wb", s)])
        return view, ("wb", s)

    def pipeline(self, units, depth=3):
        loaded = []
        n = len(units)
        for i in range(n + depth):
            if i < n:
                loaded.append(units[i][0]())
            j = i - depth
            if j >= 0:
                units[j][1](*loaded[j])
                loaded[j] = None

    def load_vec_bc(self, slot, src_row):
        wk = [("vb", slot)] + ([("ob", 0), ("ob", 1)] if slot == 1 else [])
        self.C.P.dma("sp", self.vb[slot][:], src_row.partition_broadcast(128), w=wk)

    def load_vec_fm(self, dst, key, src_row):
        self.C.P.dma("sp", dst[:], src_row.rearrange("(c p) -> p c", p=128), w=[key],
                     allow_slow_non_contiguous=True)

    def transpose_modulate(self, scp, shf, keys):
        P = self.C.P
        nt = self.nt
        g = 0
        for t in range(nt):
            for k4 in range(KC // 4):
                pb = self.pT[g % 2]
                pk = ("pT", g % 2)
                g += 1
                for j in range(4):
                    kc = k4 * 4 + j
                    P.tr(pb[:, j, :], self.xres[:, t, kc * 128:(kc + 1) * 128], self.identf[:],
                         r=[("xres", t), "identf"], w=[pk])
                for j in range(4):
                    kc = k4 * 4 + j
                    P.add("act", lambda e, pb=pb, j=j, kc=kc, t=t: e.activation(
                        self.actT[:, kc, t * 128:(t + 1) * 128], pb[:, j, :], AF.Identity,
                        bias=shf[:, kc:kc + 1], scale=scp[:, kc:kc + 1]),
                        r=[pk] + keys, w=[("actT", t)])

    def sin_turns(self, dst, dkey, off):
        P = self.C.P
        rt, ri = self.rtmp, self.rint
        P.add("dve", lambda e: e.tensor_scalar(rt[:], self.ang[:], 1.0 / (2 * PI), off, ALU.mult, ALU.add),
              r=["ang"], w=["rtmp"])
        P.add("dve", lambda e: e.tensor_copy(ri[:], rt[:]), r=["rtmp"], w=["rint"])
        P.add("dve", lambda e: e.tensor_copy(self.rflt[:], ri[:]), r=["rint"], w=["rflt"])
        P.add("dve", lambda e: e.tensor_tensor(rt[:], rt[:], self.rflt[:], ALU.subtract), r=["rtmp", "rflt"], w=["rtmp"])
        P.add("dve", lambda e: e.scalar_tensor_tensor(self.rflt[:], rt[:], 0.0, rt[:], ALU.is_lt, ALU.add),
              r=["rtmp"], w=["rflt"])
        P.add("dve", lambda e: e.tensor_scalar(rt[:], self.rflt[:], 2 * PI, -PI, ALU.mult, ALU.add), r=["rflt"], w=["rtmp"])
        P.add("dve", lambda e: e.tensor_scalar(rt[:], rt[:], 3.14159, -3.14159, ALU.min, ALU.max), r=["rtmp"], w=["rtmp"])
        P.add("act", lambda e: e.activation(dst[:], rt[:], AF.Sin), r=["rtmp"], w=[dkey])

    def layer_norm(self, t, g_slot_loader, b_slot_loader):
        P = self.C.P
        st = self.lnst
        for c in range(8):
            P.add("dve", lambda e, c=c: e.bn_stats(st[:, c, :], self.xres[:, t, c * 512:(c + 1) * 512]),
                  r=[("xres", t)], w=["lnst"])
        P.add("dve", lambda e: e.bn_aggr(self.mv[:], st[:].rearrange("p a b -> p (a b)")), r=["lnst"], w=["mv"])
        P.add("dve", lambda e: e.tensor_scalar_add(self.rstd[:], self.mv[:, 1:2], LN_EPS), r=["mv"], w=["rstd"])
        P.add("dve", lambda e: e.reciprocal(self.rstd[:], self.rstd[:]), r=["rstd"], w=["rstd"])
        P.add("act", lambda e: e.activation(self.rstd[:], self.rstd[:], AF.Sqrt), r=["rstd"], w=["rstd"])
        P.add("dve", lambda e: e.tensor_scalar(self.xres[:, t, :], self.xres[:, t, :], self.mv[:, 0:1],
                                               self.rstd[:, 0:1], ALU.subtract, ALU.mult),
              r=["mv", "rstd", ("xres", t)], w=[("xres", t)])

    def build(self):
        C = self.C
        P = C.P
        Tc, Tb, nt = self.Tc, self.Tb, self.nt
        parts = self.parts
        kinds = [p[0] for p in parts]
        layers = sorted(set(p[1] for p in parts))
        has_back = "back" in kinds
        has_front = "front" in kinds
        first_kind = kinds[0]
        io = self.io
        if has_back:
            self.Lb = [p[1] for p in parts if p[0] == "back"][0]
        if has_front:
            Lf = [p[1] for p in parts if p[0] == "front"][0]
            self.Lf = Lf
            self.fm_blocks = (list(range(0, 32)) + list(range(48, 80))) if Lf == 0 else list(range(0, 40))
            self.v_blocks = (list(range(32, 48)) + list(range(80, 96))) if Lf == 0 else list(range(40, 48))
        self.xres = C.sb("xres", [128, nt, D], F32)
        self.actT = C.sb("actT", [128, KC, Tb], BF16)
        self.nslots = 4
        self.wb = [C.sb("wb%d" % i, [128, 4096], BF16) for i in range(self.nslots)]
        self.wnext = 0
        self.vb = [C.sb("vb%d" % i, [128, D], F32) for i in range(2)]
        self.identf = C.sb("identf", [128, 128], F32)
        self.identb = C.sb("identb", [128, 128], BF16)
        self.lnst = C.sb("lnst", [128, 8, 6], F32)
        self.mv = C.sb("mv", [128, 2], F32)
        self.rstd = C.sb("rstd", [128, 1], F32)
        self.fmv = {}
        for L in layers:
            for nm in ("sc1", "sh1", "sc2", "sh2"):
                self.fmv[(L, nm)] = C.sb("fm_%s_%d" % (nm, L), [128, KC], F32)
        self.pT = [C.ps("pT%d" % i, [128, 4, 128]) for i in range(2)]
        self.pA = [C.ps("pA%d" % i, [128, 512]) for i in range(2)]
        self.pB = [C.ps("pB%d" % i, [128, 512]) for i in range(2)]
        self.pD = [C.ps("pD%d" % i, [128, 512]) for i in range(2)]
        if has_back:
            vb1b = self.vb[1][:].bitcast(BF16)
            self.ob = [vb1b[:, 0:D], vb1b[:, D:2 * D]]
            self.heT = C.sb("heT", [128, 8, Tb], BF16)
            self.sg = [C.sb("sg%d" % i, [128, Tb], F32) for i in range(2)]
            self.rw = C.sb("rw", [128, KC, NE], BF16)
            self.rb = C.sb("rb", [128, NE], F32)
            self.comb = C.sb("comb", [128, nt, NE], F32)
            self.rt = {nm: C.sb("rt_" + nm, [128, NE], F32) for nm in ("sc", "bi", "mk", "w")}
            self.rs = {nm: C.sb("rs_" + nm, [128, 8], F32) for nm in ("m1", "m2", "eq", "b2", "gs", "gm", "gk", "pen", "top", "ws")}
            self.tmp = self.sg
        if has_front:
            self.r32 = C.sb("r32", [32, 32], F32)
            self.invf = C.sb("invf_s", [32, 1], F32)
            self.posi = C.sb("posi", [32, Tb], I32)
            self.ang = C.sb("ang", [32, Tb], F32)
            self.rtmp = C.sb("rtmp", [32, Tb], F32)
            self.rint = C.sb("rint", [32, Tb], I32)
            self.rflt = C.sb("rflt", [32, Tb], F32)
            self.cosT = C.sb("cosT", [32, Tb], F32)
            self.sinT = C.sb("sinT", [32, Tb], F32)
            self.xf32 = [C.sb("xf32_0", [32, Tb], F32)] * 2
            self.t1 = [C.sb("t1_0", [32, Tb], F32)] * 2
            self.qst = [C.sb("qst%d" % i, [128, Tb], BF16) for i in range(2)]
            self.vst = [C.sb("vst%d" % i, [128, nt, 128], BF16) for i in range(2)]
            if Lf == 1:
                self.binT = C.sb("binT", [128, len(self.fm_blocks)], F32)
                self.binV = C.sb("binV", [128, len(self.v_blocks) * 128], F32)
        P.dma("sp", self.identf[:], io["ident"], w=["identf"])
        P.add("dve", lambda e: e.tensor_copy(self.identb[:], self.identf[:]), r=["identf"], w=["identb"])
        for L in layers:
            mod = io["mod"][L]
            for nm, row in (("sh1", 0), ("sc1", 1), ("sh2", 3), ("sc2", 4)):
                t = self.fmv[(L, nm)]
                self.load_vec_fm(t, ("fmv", L, nm), mod[row, :])
                if nm.startswith("sc"):
                    P.add("dve", lambda e, t=t: e.tensor_scalar_add(t[:], t[:], 1.0),
                          r=[("fmv", L, nm)], w=[("fmv", L, nm)])
        if has_back:
            P.dma("pool", self.rw[:], io["router_w"].rearrange("(c p) n -> p c n", p=128), w=["rw"],
                  allow_slow_non_contiguous=True)
            P.dma("sp", self.rb[:], io["router_b"][0, :].partition_broadcast(128), w=["rb"])
        if has_front:
            P.dma("sp", self.r32[:], io["r32t"], w=["r32"])
            P.dma("sp", self.invf[:], io["invf"], w=["invf"])
            if self.Lf == 1:
                P.dma("sp", self.binT[:], io["b_in"][0, 0:len(self.fm_blocks) * 128].rearrange("(c p) -> p c", p=128),
                      w=["binT"], allow_slow_non_contiguous=True)
                P.dma("sp", self.binV[:], io["b_in"][0, 5120:6144].partition_broadcast(128), w=["binV"])
        for blk in range(self.nblk):
            t0 = blk * Tb
            x_in_sbuf = False
            for kind, L in parts:
                if kind == "back":
                    self.back(L, t0)
                    x_in_sbuf = True
                else:
                    self.front(L, t0, x_in_sbuf)

    def qk_dst(self, bi):
        if self.Lf == 0:
            g, slot = (bi % 16) // 4, (bi // 16) * 4 + (bi % 4)
        elif bi < 32:
            g, slot = bi // 8, bi % 8
        else:
            g, slot = (bi - 32) // 2, 8 + (bi - 32) % 2
        return self.io["qk_s"][g, slot]

    def v_dst(self, vi):
        if self.Lf == 0:
            g, slot = (vi % 16) // 4, (vi // 16) * 4 + (vi % 4)
        else:
            g, slot = vi // 2, vi % 2
        return self.io["v_s"][g, :, slot * 128:(slot + 1) * 128]

    def load_o(self, ob, okeys, tok0):
        P = self.C.P
        o_r = self.io["o_r"]
        if self.Lb == 0:
            for half in range(2):
                P.dma("sp", ob[:, half * 2048:(half + 1) * 2048].rearrange("p (g c) -> p g c", g=4),
                      o_r[:, tok0:tok0 + 128, half * 512:(half + 1) * 512].rearrange("g p c -> p g c"), w=okeys)
        else:
            for g in range(4):
                P.dma("sp", ob[:, g * 1024:(g + 1) * 1024], o_r[g, tok0:tok0 + 128, :], w=okeys)

    def front(self, L, t0, x_in_sbuf):
        C, P, io = self.C, self.C.P, self.io
        Tb, nt = self.Tb, self.nt
        if not x_in_sbuf:
            for t in range(nt):
                P.dma("sp", self.xres[:, t, :], io["xin"][t0 + t * 128:t0 + (t + 1) * 128, :], w=[("xres", t)])
        self.transpose_modulate(self.fmv[(L, "sc1")], self.fmv[(L, "sh1")], [("fmv", L, "sc1"), ("fmv", L, "sh1")])
        P.dma("sp", self.posi[:], io["pos"][0, t0:t0 + Tb].partition_broadcast(32), w=["posi"])
        P.add("dve", lambda e: e.tensor_copy(self.ang[:], self.posi[:]), r=["posi"], w=["ang"])
        P.add("dve", lambda e: e.tensor_scalar(self.ang[:], self.ang[:], self.invf[:, 0:1], None, ALU.mult),
              r=["ang", "invf"], w=["ang"])
        self.sin_turns(self.sinT, "sinT", 0.5)
        self.sin_turns(self.cosT, "cosT", 0.75)
        w_in = io["w_in"]
        use_bias = (L == 1)
        units = []
        cnt = [0]

        def fm_unit(bi, cb):
            def load():
                return self.wload(w_in[:, cb * 128:(cb + 1) * 128].rearrange("(c p) n -> p c n", p=128), (KC, 128))

            def comp(wv, wk):
                i = cnt[0]
                cnt[0] += 1
                pq = self.pA[i % 2]
                pk = ("pA", i % 2)
                for kc in range(KC):
                    P.mm(pq[:, 0:Tb], wv[:, kc, :], self.actT[:, kc, :], kc == 0, kc == KC - 1,
                         r=[wk] + [("actT", t) for t in range(nt)], w=[pk])
                qs = self.qst[i % 2]
                qk = ("qst", i % 2)
                xf = self.xf32[i % 2]
                xk = ("xf32", 0)
                t1 = self.t1[i % 2]
                tk = ("t1", 0)
                pr = self.pD[i % 2]
                prk = ("pD", i % 2)
                if use_bias:
                    bia = self.binT[:, bi:bi + 1]
                    bia32 = self.binT[0:32, bi:bi + 1]
                    rk = ["binT"]
                else:
                    bia, bia32, rk = 0.0, 0.0, []
                P.add("act", lambda e: e.activation(qs[:, 0:Tb], pq[:, 0:Tb], AF.Identity, bias=bia),
                      r=[pk] + rk, w=[qk])
                P.add("act", lambda e: e.activation(xf[:], pq[0:32, 0:Tb], AF.Identity, bias=bia32),
                      r=[pk] + rk, w=[xk])
                P.mm(pr[0:32, 0:Tb], self.r32[:], xf[:], True, True, r=["r32", xk], w=[prk])
                P.add("dve", lambda e: e.tensor_tensor(t1[:], xf[:], self.cosT[:], ALU.mult), r=[xk, "cosT"], w=[tk])
                P.add("dve", lambda e: e.tensor_tensor(xf[:], pr[0:32, 0:Tb], self.sinT[:], ALU.mult),
                      r=[prk, "sinT"], w=[xk])
                P.add("dve", lambda e: e.tensor_tensor(qs[0:32, 0:Tb], t1[:], xf[:], ALU.add), r=[tk, xk, qk], w=[qk])
                P.dma("sp", self.qk_dst(bi)[:, t0:t0 + Tb], qs[:, 0:Tb], r=[qk])
            return (load, comp)

        def v_unit(vi, cb):
            def load():
                return self.wload(w_in[:, cb * 128:(cb + 1) * 128].rearrange("(c p) n -> p c n", p=128), (KC, 128))

            def comp(wv, wk):
                i = cnt[0]
                cnt[0] += 1
                pv = self.pB[i % 2]
                pk = ("pB", i % 2)
                vs = self.vst[i % 2]
                vk = ("vst", i % 2)
                for t in range(nt):
                    for kc in range(KC):
                        P.mm(pv[:, t * 128:(t + 1) * 128], self.actT[:, kc, t * 128:(t + 1) * 128], wv[:, kc, :],
                             kc == 0, kc == KC - 1, r=[wk, ("actT", t)], w=[pk])
                for t in range(nt):
                    if use_bias:
                        P.add("dve", lambda e, t=t: e.tensor_tensor(vs[:, t, :], pv[:, t * 128:(t + 1) * 128],
                                                                    self.binV[:, vi * 128:(vi + 1) * 128], ALU.add),
                              r=[pk, "binV"], w=[vk])
                    else:
                        P.add("dve", lambda e, t=t: e.tensor_copy(vs[:, t, :], pv[:, t * 128:(t + 1) * 128]), r=[pk], w=[vk])
                P.dma("sp", self.v_dst(vi)[t0:t0 + Tb, :].rearrange("(t p) n -> p t n", p=128), vs[:], r=[vk])
            return (load, comp)

        for bi, cb in enumerate(self.fm_blocks):
            units.append(fm_unit(bi, cb))
        for vi, cb in enumerate(self.v_blocks):
            units.append(v_unit(vi, cb))
        self.pipeline(units)

    def router(self, t):
        P = self.C.P
        pr = self.pT[0]
        prk = ("pT", 0)
        lg = pr[:, 0, 0:NE]
        for kc in range(KC):
            P.mm(lg, self.actT[:, kc, t * 128:(t + 1) * 128], self.rw[:, kc, :], kc == 0, kc == KC - 1,
                 r=[("actT", t), "rw"], w=[prk])
        rt, rs = self.rt, self.rs
        V = lambda nm, a, b: (rt[nm] if nm in rt else rs[nm])[:, a:b]
        seq = []

        def dve(fn, r, w):
            P.add("dve", fn, r=r, w=w)
        dve_keys = lambda *n: ["r_" + x for x in n]
        P.add("act", lambda e: e.activation(rt["sc"][:], lg, AF.Sigmoid), r=[prk], w=["r_sc"])
        dve(lambda e: e.tensor_tensor(rt["bi"][:], rt["sc"][:], self.rb[:], ALU.add), ["r_sc", "rb"], ["r_bi"])
        for g in range(4):
            bg = rt["bi"][:, 4 * g:4 * g + 4]
            dve(lambda e, bg=bg, g=g: e.tensor_reduce(rs["m1"][:, g:g + 1], bg, AX.X, ALU.max), ["r_bi"], ["r_m1"])
            dve(lambda e, bg=bg, g=g: e.tensor_scalar(rs["eq"][:, 0:4], bg, rs["m1"][:, g:g + 1], None, ALU.is_equal),
                ["r_bi", "r_m1"], ["r_eq"])
            dve(lambda e, bg=bg: e.scalar_tensor_tensor(rs["b2"][:, 0:4], rs["eq"][:, 0:4], -BIG, bg, ALU.mult, ALU.add),
                ["r_eq", "r_bi"], ["r_b2"])
            dve(lambda e, g=g: e.tensor_reduce(rs["m2"][:, g:g + 1], rs["b2"][:, 0:4], AX.X, ALU.max), ["r_b2"], ["r_m2"])
        dve(lambda e: e.tensor_tensor(rs["gs"][:, 0:4], rs["m1"][:, 0:4], rs["m2"][:, 0:4], ALU.add), ["r_m1", "r_m2"], ["r_gs"])
        dve(lambda e: e.tensor_reduce(rs["gm"][:, 0:1], rs["gs"][:, 0:4], AX.X, ALU.max), ["r_gs"], ["r_gm"])
        dve(lambda e: e.tensor_scalar(rs["gk"][:, 0:4], rs["gs"][:, 0:4], rs["gm"][:, 0:1], None, ALU.is_equal),
            ["r_gs", "r_gm"], ["r_gk"])
        dve(lambda e: e.tensor_scalar(rs["pen"][:, 0:4], rs["gk"][:, 0:4], 1.0, BIG, ALU.subtract, ALU.mult), ["r_gk"], ["r_pen"])
        for g in range(4):
            dve(lambda e, g=g: e.tensor_scalar(rt["mk"][:, 4 * g:4 * g + 4], rt["bi"][:, 4 * g:4 * g + 4],
                                               rs["pen"][:, g:g + 1], None, ALU.add), ["r_bi", "r_pen"], ["r_mk"])
        dve(lambda e: e.max(rs["top"][:, 0:8], rt["mk"][:]), ["r_mk"], ["r_top"])
        dve(lambda e: e.tensor_scalar(rt["w"][:], rt["mk"][:], rs["top"][:, 1:2], None, ALU.is_ge), ["r_mk", "r_top"], ["r_w"])
        dve(lambda e: e.tensor_tensor(rt["w"][:], rt["w"][:], rt["sc"][:], ALU.mult), ["r_w", "r_sc"], ["r_w"])
        dve(lambda e: e.tensor_reduce(rs["ws"][:, 0:1], rt["w"][:], AX.X, ALU.add), ["r_w"], ["r_ws"])
        dve(lambda e: e.reciprocal(rs["ws"][:, 1:2], rs["ws"][:, 0:1]), ["r_ws"], ["r_ws"])
        dve(lambda e: e.tensor_scalar(self.comb[:, t, :], rt["w"][:], rs["ws"][:, 1:2], None, ALU.mult),
            ["r_w", "r_ws"], [("comb", t)])

    def back(self, L, t0):
        C, P, io = self.C, self.C.P, self.io
        Tb, nt = self.Tb, self.nt
        mod = io["mod"][L]
        g = 0
        for t in range(nt):
            ob = self.ob[t % 2]
            self.load_o(ob, [("ob", t % 2), ("vb", 1)], t0 + t * 128)
            for k8 in range(KC // 8):
                pb = self.pT[g % 2]
                pk = ("pT", g % 2)
                g += 1
                pbb = pb[:].rearrange("p a b -> p (a b)").bitcast(BF16)
                for j in range(8):
                    kc = k8 * 8 + j
                    P.tr(pbb[:, j * 128:(j + 1) * 128], ob[:, kc * 128:(kc + 1) * 128], self.identb[:],
                         r=[("ob", t % 2), "identb"], w=[pk])
                P.add("act", lambda e, pbb=pbb, k8=k8, t=t: e.activation(
                    self.actT[:, k8 * 8:(k8 + 1) * 8, t * 128:(t + 1) * 128],
                    pbb.rearrange("p (a b) -> p a b", b=128), AF.Identity), r=[pk], w=[("actT", t)])
        for t in range(nt):
            P.dma("sp", self.xres[:, t, :], io["xin"][t0 + t * 128:t0 + (t + 1) * 128, :], w=[("xres", t)])
        self.load_vec_bc(0, mod[2, :])
        w_out = io["w_out"]
        cnt = [0]

        def wo_unit(cb):
            def load():
                return self.wload(w_out[:, cb * 128:(cb + 1) * 128].rearrange("(c p) n -> p c n", p=128), (KC, 128))

            def comp(wv, wk):
                i = cnt[0]
                cnt[0] += 1
                pv = self.pB[i % 2]
                pk = ("pB", i % 2)
                tm = self.tmp[i % 2]
                tk = ("sg", i % 2)
                for t in range(nt):
                    for kc in range(KC):
                        P.mm(pv[:, t * 128:(t + 1) * 128], self.actT[:, kc, t * 128:(t + 1) * 128], wv[:, kc, :],
                             kc == 0, kc == KC - 1, r=[wk, ("actT", t)], w=[pk])
                for t in range(nt):
                    xs = self.xres[:, t, cb * 128:(cb + 1) * 128]
                    P.add("dve", lambda e, t=t: e.tensor_tensor(tm[:, t * 128:(t + 1) * 128], pv[:, t * 128:(t + 1) * 128],
                                                                self.vb[0][:, cb * 128:(cb + 1) * 128], ALU.mult),
                          r=[pk, ("vb", 0)], w=[tk])
                    P.add("dve", lambda e, t=t, xs=xs: e.scalar_tensor_tensor(xs, xs, ALPHA, tm[:, t * 128:(t + 1) * 128],
                                                                              ALU.mult, ALU.add),
                          r=[tk, ("xres", t)], w=[("xres", t)])
            return (load, comp)
        self.pipeline([wo_unit(cb) for cb in range(KC)])
        self.load_vec_bc(1, io["ln"][0, :])
        for t in range(nt):
            self.layer_norm(t, None, None)
            P.add("dve", lambda e, t=t: e.tensor_tensor(self.xres[:, t, :], self.xres[:, t, :], self.vb[1][:], ALU.mult),
                  r=[("xres", t), ("vb", 1)], w=[("xres", t)])
        self.load_vec_bc(0, io["ln"][1, :])
        for t in range(nt):
            P.add("dve", lambda e, t=t: e.tensor_tensor(self.xres[:, t, :], self.xres[:, t, :], self.vb[0][:], ALU.add),
                  r=[("xres", t), ("vb", 0)], w=[("xres", t)])
            P.dma("sp", io["x1scr"][t0 + t * 128:t0 + (t + 1) * 128, :], self.xres[:, t, :], r=[("xres", t)],
                  w=[("x1scr", t0, t)])
        self.transpose_modulate(self.fmv[(L, "sc2")], self.fmv[(L, "sh2")], [("fmv", L, "sc2"), ("fmv", L, "sh2")])
        for t in range(nt):
            self.router(t)
        wg, wu, wd = io["w_gate"], io["w_up"], io["w_down"]
        units = []
        cnt = [0]
        dcnt = [0]

        def gu_unit(e_, fc):
            def load():
                a = self.wload(wg[e_, :, fc * 128:(fc + 1) * 128].rearrange("(c p) n -> p c n", p=128), (KC, 128))
                b = self.wload(wu[e_, :, fc * 128:(fc + 1) * 128].rearrange("(c p) n -> p c n", p=128), (KC, 128))
                return a + b

            def comp(gv, gk, uv, uk):
                i = cnt[0]
                cnt[0] += 1
                pg, pgk = self.pA[i % 2], ("pA", i % 2)
                pu, puk = self.pB[i % 2], ("pB", i % 2)
                sg, sgk = self.sg[i % 2], ("sg", i % 2)
                ak = [("actT", t) for t in range(nt)]
                for kc in range(KC):
                    P.mm(pg[:, 0:Tb], gv[:, kc, :], self.actT[:, kc, :], kc == 0, kc == KC - 1, r=[gk] + ak, w=[pgk])
                for kc in range(KC):
                    P.mm(pu[:, 0:Tb], uv[:, kc, :], self.actT[:, kc, :], kc == 0, kc == KC - 1, r=[uk] + ak, w=[puk])
                P.add("act", lambda e: e.activation(sg[:, 0:Tb], pg[:, 0:Tb], AF.Silu), r=[pgk], w=[sgk])
                P.add("dve", lambda e: e.tensor_tensor(self.heT[:, fc, :], sg[:, 0:Tb], pu[:, 0:Tb], ALU.mult),
                      r=[sgk, puk], w=[("heT", fc)])
            return (load, comp)

        def d_unit(e_, dc):
            def load():
                return self.wload(wd[e_, :, dc * 512:(dc + 1) * 512].rearrange("(c p) n -> p c n", p=128), (8, 512))

            def comp(wv, wk):
                for t in range(nt):
                    i = dcnt[0]
                    dcnt[0] += 1
                    pd, pdk = self.pD[i % 2], ("pD", i % 2)
                    for fc in range(8):
                        P.mm(pd[:], self.heT[:, fc, t * 128:(t + 1) * 128], wv[:, fc, :], fc == 0, fc == 7,
                             r=[wk, ("heT", fc)], w=[pdk])
                    xs = self.xres[:, t, dc * 512:(dc + 1) * 512]
                    cw = self.comb[:, t, e_:e_ + 1]
                    if e_ == 0:
                        P.add("dve", lambda e, pd=pd, xs=xs, cw=cw: e.tensor_scalar(xs, pd[:], cw, None, ALU.mult),
                              r=[pdk, ("comb", t)], w=[("xres", t)])
                    else:
                        P.add("dve", lambda e, pd=pd, xs=xs, cw=cw: e.scalar_tensor_tensor(xs, pd[:], cw, xs, ALU.mult, ALU.add),
                              r=[pdk, ("comb", t), ("xres", t)], w=[("xres", t)])
            return (load, comp)
        for e_ in range(NE):
            for fc in range(8):
                units.append(gu_unit(e_, fc))
            for dc in range(8):
                units.append(d_unit(e_, dc))
        self.pipeline(units, depth=1)
        self.load_vec_bc(1, mod[5, :])
        for t in range(nt):
            P.add("dve", lambda e, t=t: e.tensor_tensor(self.xres[:, t, :], self.xres[:, t, :], self.vb[1][:], ALU.mult),
                  r=[("xres", t), ("vb", 1)], w=[("xres", t)])
        for t in range(nt):
            P.dma("sp", self.vb[0][:], io["x1scr"][t0 + t * 128:t0 + (t + 1) * 128, :], r=[("x1scr", t0, t)], w=[("vb", 0)])
            P.add("dve", lambda e, t=t: e.scalar_tensor_tensor(self.xres[:, t, :], self.vb[0][:], ALPHA, self.xres[:, t, :],
                                                               ALU.mult, ALU.add),
                  r=[("xres", t), ("vb", 0)], w=[("xres", t)])
            self.layer_norm(t, None, None)
        self.load_vec_bc(1, io["ln"][2, :])
        for t in range(nt):
            P.add("dve", lambda e, t=t: e.tensor_tensor(self.xres[:, t, :], self.xres[:, t, :], self.vb[1][:], ALU.mult),
                  r=[("xres", t), ("vb", 1)], w=[("xres", t)])
        self.load_vec_bc(0, io["ln"][3, :])
        for t in range(nt):
            P.add("dve", lambda e, t=t: e.tensor_tensor(self.xres[:, t, :], self.xres[:, t, :], self.vb[0][:], ALU.add),
                  r=[("xres", t), ("vb", 0)], w=[("xres", t)])
            P.dma("sp", io["xout"][t0 + t * 128:t0 + (t + 1) * 128, :], self.xres[:, t, :], r=[("xres", t)])


def _mask_consts(S):
    k = np.arange(128)[:, None]
    q = np.arange(256)[None, :]
    md = np.stack([np.where(q >= k + 128 * r, 0.0, -BIG) for r in range(2)]).astype(np.float32)
    q = np.arange(512)[None, :]
    mm_ = []
    for r in range(4):
        same = (q // 256) == (r // 2)
        caus = np.where(q >= k + 128 * r, 0.0, -BIG)
        mm_.append(np.where(same, caus, 0.0))
    mm_ = np.stack(mm_).astype(np.float32)
    q = np.arange(128)[None, :]
    ms = np.stack([np.tile(np.where(q >= k, 0.0, -BIG), (1, 4)), np.tile(np.where(k > q, 0.0, -BIG), (1, 4))]).astype(np.float32)
    E = np.zeros((32, 32, 128), np.float32)
    for j in range(32):
        E[j, j, :] = 1.0
    E = E.transpose(1, 0, 2).reshape(32, 32 * 128)
    return md, mm_, ms, E


def stage_attn0(C, S, io):
    P = C.P
    NT = S // 128
    NB = S // 256
    assert NB <= 32
    qk_r = io["qk_r"].rearrange("(g s p) t -> g s p t", g=4, p=128)
    v_r = io["v_r"]
    o_out = io["o_s"]
    lamv, subg, ident_d, md_d, mm_d, E_d = io["lamv"], io["subg"], io["ident"], io["md"], io["mm"], io["E"]

    def fm(slot):
        return qk_r[:, slot].rearrange("r p t -> p r t")

    def sv(t):
        return t[:].rearrange("p (r t) -> p r t", r=4)
    identf = C.sb("identf", [128, 128], F32)
    identb = C.sb("identb", [128, 128], BF16)
    mdb = C.sb("mdb", [128, 2, 256], BF16)
    mmb = C.sb("mmb", [128, 4, 512], BF16)
    Eb = C.sb("Eb", [32, 32 * 128], BF16)
    lam_s = C.sb("lam_s", [128, 4, 128], F32)
    lamt = C.sb("lamt", [128, 8], F32)
    gsub = C.sb("gsub", [128, 256], F32)
    QA = [C.sb("QA%d" % m, [128, S], BF16) for m in range(2)]
    KA = [C.sb("KA%d" % m, [128, S], BF16) for m in range(2)]
    VA = C.sb("VA", [128, NT, 257], BF16)
    QB = C.sb("QB", [128, S], BF16)
    KB = C.sb("KB", [128, S], BF16)
    VB = C.sb("VB", [128, NT, 129], BF16)
    negT = C.sb("negT", [32, S], BF16)
    kmean = C.sb("kmean", [128, 32], F32)
    kmeanb = C.sb("kmeanb", [128, 32], BF16)
    gate = C.sb("gate", [128, 32], F32)
    negm = C.sb("negm", [128, 32], F32)
    top8 = C.sb("top8", [128, 8], F32)
    PT = [C.sb("PT%d" % i, [128, 512], BF16) for i in range(2)]
    fin = {nm: C.sb("fin_" + nm, [128, 4], F32) for nm in ("r1", "r2", "ss", "rs")}
    ot = [C.sb("ot%d" % i, [128, 256], F32) for i in range(2)]
    ou = [C.sb("ou%d" % i, [128, 256], F32) for i in range(2)]
    junk = C.sb("junk", [128, 256], BF16)
    ost = [C.sb("ost%d" % i, [128, 256], BF16) for i in range(2)]
    stage = C.sb("stage", [128, 512], F32)
    pS = [C.ps("pS%d" % i, [128, 512]) for i in range(2)]
    pO = [C.ps("pO%d" % i, [128, 512]) for i in range(4)]
    pG = C.ps("pG", [128, 512])
    pX = C.ps("pX", [128, 512])
    P.dma("sp", identf[:], ident_d, w=["identf"])
    P.add("dve", lambda e: e.tensor_copy(identb[:], identf[:]), r=["identf"], w=["identb"])
    for r_ in range(2):
        P.dma("pool", mdb[:, r_, :], md_d[r_], w=["mdb"])
    for r_ in range(4):
        P.dma("pool", mmb[:, r_, :], mm_d[r_], w=["mmb"])
    P.dma("pool", Eb[:], E_d, w=["Eb"])
    for i in range(4):
        P.dma("sp", lam_s[:, i, :], lamv[i, :].partition_broadcast(128), w=[("lam_s", i)])
    P.dma("sp", gsub[:], subg[0, :].partition_broadcast(128), w=["gsub"])
    P.add("dve", lambda e: e.tensor_tensor(lam_s[:, 0, :], lam_s[:, 0, :], lam_s[:, 1, :], ALU.mult),
          r=[("lam_s", 0), ("lam_s", 1)], w=[("lam_s", 0)])
    P.add("dve", lambda e: e.tensor_tensor(lam_s[:, 2, :], lam_s[:, 2, :], lam_s[:, 3, :], ALU.mult),
          r=[("lam_s", 2), ("lam_s", 3)], w=[("lam_s", 2)])
    P.add("dve", lambda e: e.tensor_reduce(lamt[:, 0:1], lam_s[:, 0, :], AX.X, ALU.add), r=[("lam_s", 0)], w=["lamt"])
    P.add("dve", lambda e: e.tensor_reduce(lamt[:, 1:2], lam_s[:, 2, :], AX.X, ALU.add), r=[("lam_s", 2), "lamt"], w=["lamt"])
    P.add("act", lambda e: e.activation(lamt[:, 2:4], lamt[:, 0:2], AF.Exp), r=["lamt"], w=["lamt2"])
    P.add("dve", lambda e: e.tensor_tensor(lamt[:, 4:5], lamt[:, 2:3], lamt[:, 3:4], ALU.subtract), r=["lamt2"], w=["lamt3"])
    P.add("dve", lambda e: e.tensor_scalar(lamt[:, 5:6], lamt[:, 4:5], LAM_INIT, -1.0, ALU.add, ALU.mult),
          r=["lamt3"], w=["neglam"])
    P.add("dve", lambda e: e.tensor_scalar_mul(gsub[:], gsub[:], 1.0 - LAM_INIT), r=["gsub"], w=["gsub"])
    nstep = [0]

    for h in range(2):
        for m in range(2):
            P.dma("sp", sv(QA[m]), fm(2 * h + m), w=[("QA", m)])
            P.dma("sp", sv(KA[m]), fm(4 + 2 * h + m), w=[("KA", m)])
        P.add("pool", lambda e: e.memset(VA[:, :, 256:257], 1.0), w=["VA"])
        P.dma("sp", VA[:, :, 0:256], v_r[:, h * 256:(h + 1) * 256].rearrange("(t p) n -> p t n", p=128), w=["VA"])
        for qc in range(S // 256):
            nk = 2 * qc + 2
            for kt in range(nk):
                i = nstep[0]
                nstep[0] += 1
                ps, psk = pS[i % 2], ("pS", i % 2)
                pt, ptk = PT[i % 2], ("PT", i % 2)
                diag = kt >= 2 * qc
                for m in range(2):
                    P.mm(ps[:, m * 256:(m + 1) * 256], KA[m][:, kt * 128:(kt + 1) * 128], QA[m][:, qc * 256:(qc + 1) * 256],
                         True, not diag, r=[("KA", m), ("QA", m)], w=[psk])
                    if diag:
                        P.mm(ps[:, m * 256:(m + 1) * 256], identb[:], mdb[:, kt - 2 * qc, :], False, True,
                             r=["identb", "mdb"], w=[psk])
                P.add("act", lambda e, ps=ps, pt=pt: e.activation(pt[:], ps[:], AF.Exp, scale=SCALE), r=[psk], w=[ptk])
                for m in range(2):
                    for t in range(2):
                        if kt <= 2 * qc + t:
                            P.mm(pO[m * 2 + t][:, 0:257], pt[:, m * 256 + t * 128:m * 256 + (t + 1) * 128], VA[:, kt, :],
                                 kt == 0, kt == 2 * qc + t, r=[ptk, "VA"], w=[("pO", m * 2 + t)])
            for t in range(2):
                j = t
                o1, o2 = pO[t], pO[2 + t]
                k1, k2 = ("pO", t), ("pO", 2 + t)
                fk = ("fin", j)
                P.add("dve", lambda e, o1=o1, j=j: e.reciprocal(fin["r1"][:, j:j + 1], o1[:, 256:257]), r=[k1], w=[fk + ("r1",)])
                P.add("dve", lambda e, o2=o2, j=j: e.reciprocal(fin["r2"][:, j:j + 1], o2[:, 256:257]), r=[k2], w=[fk + ("r2",)])
                P.add("dve", lambda e, j=j: e.tensor_tensor(fin["r2"][:, j:j + 1], fin["r2"][:, j:j + 1], lamt[:, 5:6], ALU.mult),
                      r=[fk + ("r2",), "neglam"], w=[fk + ("r2",)])
                P.add("dve", lambda e, o1=o1, j=j: e.tensor_scalar(ot[j][:], o1[:, 0:256], fin["r1"][:, j:j + 1], None, ALU.mult),
                      r=[k1, fk + ("r1",)], w=[("ot", j)])
                P.add("dve", lambda e, o2=o2, j=j: e.scalar_tensor_tensor(ou[j][:], o2[:, 0:256], fin["r2"][:, j:j + 1], ot[j][:],
                                                                          ALU.mult, ALU.add),
                      r=[k2, fk + ("r2",), ("ot", j)], w=[("ou", j)])
                P.add("dve", lambda e, j=j: e.memset(fin["ss"][:, j:j + 1], 0.0), w=[fk + ("ss",)])
                P.add("act", lambda e, j=j: e.activation(junk[:], ou[j][:], AF.Square, accum_out=fin["ss"][:, j:j + 1]),
                      r=[("ou", j), fk + ("ss",)], w=["junk", fk + ("ss",)])
                P.add("dve", lambda e, j=j: e.tensor_scalar(fin["rs"][:, j:j + 1], fin["ss"][:, j:j + 1], 1.0 / 256.0, RMS_EPS,
                                                            ALU.mult, ALU.add), r=[fk + ("ss",)], w=[fk + ("rs",)])
                P.add("dve", lambda e, j=j: e.reciprocal(fin["rs"][:, j:j + 1], fin["rs"][:, j:j + 1]),
                      r=[fk + ("rs",)], w=[fk + ("rs",)])
                P.add("act", lambda e, j=j: e.activation(fin["rs"][:, j:j + 1], fin["rs"][:, j:j + 1], AF.Sqrt),
                      r=[fk + ("rs",)], w=[fk + ("rs",)])
                P.add("dve", lambda e, j=j: e.scalar_tensor_tensor(ost[j][:], ou[j][:], fin["rs"][:, j:j + 1], gsub[:],
                                                                   ALU.mult, ALU.mult),
                      r=[("ou", j), fk + ("rs",), "gsub"], w=[("ost", j)])
                tq = 2 * qc + t
                P.dma("sp", o_out[tq * 128:(tq + 1) * 128, h * 256:(h + 1) * 256], ost[j][:], r=[("ost", j)])

    for h in range(4):
        P.dma("sp", sv(QB), fm(8 + h), w=["QB"])
        P.dma("sp", sv(KB), fm(12 + h), w=["KB"])
        P.add("pool", lambda e: e.memset(VB[:, :, 128:129], 1.0), w=["VB"])
        P.dma("sp", VB[:, :, 0:128], v_r[:, 512 + h * 128:512 + (h + 1) * 128].rearrange("(t p) n -> p t n", p=128), w=["VB"])
        P.add("dve", lambda e: e.tensor_reduce(kmean[:, 0:NB], KB[:].rearrange("p (n k) -> p n k", k=256), AX.X, ALU.add),
              r=["KB"], w=["kmean"])
        P.add("dve", lambda e: e.tensor_scalar_mul(kmeanb[:, 0:NB], kmean[:, 0:NB], 1.0 / 256.0), r=["kmean"], w=["kmeanb"])
        for t in range(NT):
            own = t // 2
            P.add("dve", lambda e: e.memset(negm[:], -BIG), w=["negm"])
            if own >= 1:
                if own > 3:
                    P.mm(pG[:, 0:NB], QB[:, t * 128:(t + 1) * 128], kmeanb[:, 0:NB], True, True, r=["QB", "kmeanb"], w=["pG"])
                    P.add("dve", lambda e: e.memset(gate[:], -BIG), w=["gate"])
                    P.add("dve", lambda e, own=own: e.tensor_copy(gate[:, 0:own], pG[:, 0:own]), r=["pG", "gate"], w=["gate"])
                    P.add("dve", lambda e: e.max(top8[:], gate[:]), r=["gate"], w=["top8"])
                    P.add("dve", lambda e, own=own: e.tensor_scalar(negm[:, 0:own], gate[:, 0:own], top8[:, 2:3], None, ALU.is_ge),
                          r=["gate", "top8", "negm"], w=["negm"])
                    P.add("dve", lambda e, own=own: e.tensor_scalar(negm[:, 0:own], negm[:, 0:own], 1.0, BIG, ALU.subtract, ALU.mult),
                          r=["negm"], w=["negm"])
                else:
                    P.add("dve", lambda e, own=own: e.memset(negm[:, 0:own], 0.0), r=["negm"], w=["negm"])
            P.add("dve", lambda e, own=own: e.memset(negm[:, own:own + 1], 0.0), r=["negm"], w=["negm"])
            P.tr(pX[0:32, 0:128], negm[:], identf[:], r=["negm", "identf"], w=["pX"])
            P.add("act", lambda e, t=t: e.activation(negT[:, t * 128:(t + 1) * 128], pX[0:32, 0:128], AF.Identity),
                  r=["pX"], w=["negT"])
        for c in range(S // 512):
            nk = 4 * c + 4
            for kt in range(nk):
                i = nstep[0]
                nstep[0] += 1
                ps, psk = pS[i % 2], ("pS", i % 2)
                pt, ptk = PT[i % 2], ("PT", i % 2)
                j = kt // 2
                P.mm(ps[:], KB[:, kt * 128:(kt + 1) * 128], QB[:, c * 512:(c + 1) * 512], True, False, r=["KB", "QB"], w=[psk])
                last_sel = kt < 4 * c
                P.mm(ps[:], Eb[:, j * 128:(j + 1) * 128], negT[:, c * 512:(c + 1) * 512], False, last_sel,
                     r=["Eb", "negT"], w=[psk])
                if not last_sel:
                    P.mm(ps[:], identb[:], mmb[:, kt - 4 * c, :], False, True, r=["identb", "mmb"], w=[psk])
                P.add("act", lambda e, ps=ps, pt=pt: e.activation(pt[:], ps[:], AF.Exp, scale=SCALE), r=[psk], w=[ptk])
                for tq in range(4):
                    if kt <= 4 * c + tq:
                        P.mm(pO[tq][:, 0:129], pt[:, tq * 128:(tq + 1) * 128], VB[:, kt, :], kt == 0, kt == 4 * c + tq,
                             r=[ptk, "VB"], w=[("pO", tq)])
            for tq in range(4):
                j = tq % 2
                fk = ("fin", j)
                P.add("dve", lambda e, tq=tq, j=j: e.reciprocal(fin["r1"][:, j:j + 1], pO[tq][:, 128:129]),
                      r=[("pO", tq)], w=[fk + ("r1",)])
                P.add("dve", lambda e, tq=tq, j=j: e.tensor_scalar(ost[j][:, 0:128], pO[tq][:, 0:128], fin["r1"][:, j:j + 1], None, ALU.mult),
                      r=[("pO", tq), fk + ("r1",)], w=[("ost", j)])
                tt = 4 * c + tq
                P.dma("sp", o_out[tt * 128:(tt + 1) * 128, 512 + h * 128:512 + (h + 1) * 128], ost[j][:, 0:128], r=[("ost", j)])


def stage_attn1(C, S, io):
    P = C.P
    NT = S // 128
    qk_r = io["qk_r"].rearrange("(g s p) t -> g s p t", g=4, p=128)
    v_d = io["v_r"]
    o_out = io["o_s"]
    sk_d, ident_d, ms_d = io["sinks"], io["ident"], io["ms"]

    def fm(slot):
        return qk_r[:, slot].rearrange("r p t -> p r t")
    identf = C.sb("identf", [128, 128], F32)
    identb = C.sb("identb", [128, 128], BF16)
    msb = C.sb("msb", [128, 2, 512], BF16)
    esink = C.sb("esink", [128, 8], F32)
    Q = C.sb("Q", [128, 4, S], BF16)
    K = C.sb("K", [128, S], BF16)
    V = C.sb("V", [128, NT, 129], BF16)
    PT = [C.sb("PT%d" % i, [128, 512], BF16) for i in range(2)]
    den = C.sb("den", [128, 8], F32)
    ost = [C.sb("ost%d" % i, [128, 512], BF16) for i in range(2)]
    pS = [C.ps("pS%d" % i, [128, 512]) for i in range(2)]
    pO = [C.ps("pO%d" % i, [128, 512]) for i in range(4)]
    P.dma("sp", identf[:], ident_d, w=["identf"])
    P.add("dve", lambda e: e.tensor_copy(identb[:], identf[:]), r=["identf"], w=["identb"])
    for r_ in range(2):
        P.dma("pool", msb[:, r_, :], ms_d[r_], w=["msb"])
    P.dma("sp", esink[:], sk_d[0, :].partition_broadcast(128), w=["esink"])
    P.add("act", lambda e: e.activation(esink[:], esink[:], AF.Exp), r=["esink"], w=["esink"])
    n = 0
    for j in range(2):
        for hh in range(4):
            P.dma("sp", Q[:, hh, :].rearrange("p (r t) -> p r t", r=4), fm(4 * j + hh), w=["Q"])
        P.dma("sp", K[:].rearrange("p (r t) -> p r t", r=4), fm(8 + j), w=["K"])
        P.add("pool", lambda e: e.memset(V[:, :, 128:129], 1.0), w=["V"])
        P.dma("sp", V[:, :, 0:128], v_d[:, j * 128:(j + 1) * 128].rearrange("(t p) n -> p t n", p=128), w=["V"])
        for t in range(NT):
            kts = [t - 1, t] if t > 0 else [t]
            for kt in kts:
                ps, psk = pS[n % 2], ("pS", n % 2)
                pt, ptk = PT[n % 2], ("PT", n % 2)
                n += 1
                P.mm(ps[:].rearrange("p (h q) -> p h q", q=128), K[:, kt * 128:(kt + 1) * 128], Q[:, :, t * 128:(t + 1) * 128],
                     True, False, r=["K", "Q"], w=[psk])
                P.mm(ps[:], identb[:], msb[:, 0 if kt == t else 1, :], False, True, r=["identb", "msb"], w=[psk])
                P.add("act", lambda e, ps=ps, pt=pt: e.activation(pt[:], ps[:], AF.Exp, scale=SCALE), r=[psk], w=[ptk])
                for hh in range(4):
                    P.mm(pO[hh][:, 0:129], pt[:, hh * 128:(hh + 1) * 128], V[:, kt, :], kt == kts[0], kt == t,
                         r=[ptk, "V"], w=[("pO", hh)])
            os_, osk = ost[t % 2], ("ost", t % 2)
            for hh in range(4):
                hq = 4 * j + hh
                P.add("dve", lambda e, hh=hh, hq=hq: e.tensor_tensor(den[:, hh:hh + 1], pO[hh][:, 128:129], esink[:, hq:hq + 1], ALU.add),
                      r=[("pO", hh), "esink"], w=[("den", hh)])
                P.add("dve", lambda e, hh=hh: e.reciprocal(den[:, 4 + hh:5 + hh], den[:, hh:hh + 1]), r=[("den", hh)], w=[("den", hh)])
                P.add("dve", lambda e, hh=hh, os_=os_: e.tensor_scalar(os_[:, hh * 128:(hh + 1) * 128], pO[hh][:, 0:128],
                                                                        den[:, 4 + hh:5 + hh], None, ALU.mult),
                      r=[("pO", hh), ("den", hh)], w=[osk])
            P.dma("sp", o_out[t * 128:(t + 1) * 128, j * 512:(j + 1) * 512], os_[:], r=[osk])


def _r0(rows, L):
    return min(rows, max(128, (1 << 20) // (L * 2)))


def cc_chunks(P, s_ap, g_ap):
    rows, L = s_ap.shape[0], s_ap.shape[1]
    R0 = _r0(rows, L)
    assert rows % R0 == 0
    for c in range(rows // R0):
        P.cc("AllGather", s_ap[c * R0:(c + 1) * R0, :], g_ap[c * 4 * R0:(c + 1) * 4 * R0, :], GROUPS)


def _tab(g, nblk, R0):
    cols = []
    for src in range(4):
        for k in range(nblk):
            lr0 = (g * nblk + k) * 128
            cols.append(((lr0 // R0) * 4 + src) * R0 + lr0 % R0)
    return np.asarray(cols, np.int64)[None, :] + np.arange(128, dtype=np.int64)[:, None]


def stage_select(C, idxtab_d, ncol_total, jobs):
    P = C.P
    ix = C.sb("ix", [128, ncol_total], I32)
    P.dma("sp", ix[:], idxtab_d, w=["ix"])
    maxL = max(job[0].shape[1] for job in jobs)
    stg = [C.sb("stg%d" % i, [128, maxL], BF16) for i in range(4)]
    n = 0
    for src, dst, col0, ncols in jobs:
        L = src.shape[1]
        for j in range(ncols):
            s_ = n % 4
            n += 1
            P.gather(stg[s_][:, 0:L], src, ix[:, col0 + j:col0 + j + 1], r=["ix"], w=[("stg", s_)])
            P.dma("sp", dst[j * 128:(j + 1) * 128, :], stg[s_][:, 0:L], r=[("stg", s_)])


def build_all(S, cut=6, dbg=False):
    C = Ctx()
    P = C.P
    Tc = S // 4
    NTT = S // 128
    NIDX = 104 + 2 * NTT
    spec = dict(
        ident=([128, 128], F32), r32t=([32, 32], F32), invf=([32, 1], F32),
        md=([2, 128, 256], F32), mm=([4, 128, 512], F32), ms=([2, 128, 512], F32), E=([32, 32 * 128], F32),
        xin=([Tc, D], F32), pos=([1, Tc], I32), c_own=([1, D], F32), idxtab=([128, NIDX], I32),
        wada0=([D, 6144], F32), bada0=([1, 6144], F32), wada1=([D, 6144], F32), bada1=([1, 6144], F32),
        router_w=([D, NE], F32), router_b=([1, NE], F32),
        w_in0=([D, 12288], F32), lamv=([4, 128], F32), subg=([1, 256], F32), w_out0=([D, D], F32),
        ln0=([4, D], F32), wg0=([NE, D, DFF], F32), wu0=([NE, D, DFF], F32), wd0=([NE, DFF, D], F32),
        w_in1=([D, 6144], F32), b_in1=([1, 6144], F32), sinks=([1, 8], F32), w_out1=([D, D], F32),
        ln1=([4, D], F32), wg1=([NE, D, DFF], F32), wu1=([NE, D, DFF], F32), wd1=([NE, DFF, D], F32))
    decl = {}
    outs = []

    def X(nm):
        if nm not in decl:
            decl[nm] = C.din(nm, spec[nm][0], spec[nm][1])
        return decl[nm]

    def scr(nm, shape, dt, out_at=None):
        if dbg and out_at is not None and cut in (out_at if isinstance(out_at, tuple) else (out_at,)):
            outs.append(nm)
            return C.dout(nm, shape, dt)
        return C.dscr(nm, shape, dt)

    ada_in = scr("ada_in", [2, 6144], F32)
    ada_g = scr("ada_g", [8, 6144], F32)
    mod = scr("mod", [2, 6, D], F32, (1, 1.7))
    x1scr = scr("x1scr", [Tc, D], F32)
    x2scr = scr("x2scr", [Tc, D], F32, 4)
    qk0_s = scr("qk0_s", [4 * 16 * 128, Tc], BF16, 1.5)
    qk0_g = scr("qk0_g", [16 * 16 * 128, Tc], BF16)
    qk0_r = scr("qk0_r", [4 * 16 * 128, Tc], BF16, 2)
    v0_s = scr("v0_s", [4 * Tc, 1024], BF16, 1.5)
    v0_g = scr("v0_g", [16 * Tc, 1024], BF16)
    v0_r = scr("v0_r", [4 * Tc, 1024], BF16, 2)
    o0_s = scr("o0_s", [4 * Tc, 1024], BF16)
    o0_g = scr("o0_g", [16 * Tc, 1024], BF16)
    o0_r = scr("o0_r", [4 * Tc, 1024], BF16, 3)
    qk1_s = scr("qk1_s", [4 * 10 * 128, Tc], BF16)
    qk1_g = scr("qk1_g", [16 * 10 * 128, Tc], BF16)
    qk1_r = scr("qk1_r", [4 * 10 * 128, Tc], BF16, 4)
    v1_s = scr("v1_s", [4 * Tc, 256], BF16)
    v1_g = scr("v1_g", [16 * Tc, 256], BF16)
    v1_r = scr("v1_r", [4 * Tc, 256], BF16, 4)
    o1_s = scr("o1_s", [4 * Tc, 1024], BF16)
    o1_g = scr("o1_g", [16 * Tc, 1024], BF16)
    o1_r = scr("o1_r", [4 * Tc, 1024], BF16, 5)
    v4 = lambda ap: ap.rearrange("(g s p) t -> g s p t", g=4, p=128)
    v3 = lambda ap: ap.rearrange("(g t) c -> g t c", g=4)

    def common():
        return dict(ident=X("ident"), mod=mod, r32t=X("r32t"), invf=X("invf"), pos=X("pos"), x1scr=x1scr)

    def done():
        nc = C.finish()
        return nc, sorted(decl), outs

    stage_ada(C, dict(c_own=X("c_own"), wada0=X("wada0"), bada0=X("bada0"), wada1=X("wada1"), bada1=X("bada1"),
                      ada_in=ada_in, ada_g=ada_g, mod=mod))
    if cut == 1:
        return done()
    C.new_stage()
    TokStage(C, Tc, [("front", 0)], dict(common(), xin=X("xin"), w_in=X("w_in0"), qk_s=v4(qk0_s), v_s=v3(v0_s)))
    if cut == 1.5:
        return done()
    cc_chunks(P, qk0_s, qk0_g)
    cc_chunks(P, v0_s, v0_g)
    if cut == 1.7:
        return done()
    C.new_stage()
    stage_select(C, X("idxtab"), NIDX, [(qk0_g, qk0_r, 0, 64), (v0_g, v0_r, 64, NTT)])
    if cut == 2:
        return done()
    C.new_stage()
    stage_attn0(C, S, dict(qk_r=qk0_r, v_r=v0_r, o_s=o0_s, lamv=X("lamv"), subg=X("subg"), ident=X("ident"),
                           md=X("md"), mm=X("mm"), E=X("E")))
    cc_chunks(P, o0_s, o0_g)
    C.new_stage()
    stage_select(C, X("idxtab"), NIDX, [(o0_g, o0_r, 64, NTT)])
    if cut == 3:
        return done()
    C.new_stage()
    TokStage(C, Tc, [("back", 0), ("front", 1)],
             dict(common(), router_w=X("router_w"), router_b=X("router_b"), xin=X("xin"), o_r=v3(o0_r), w_out=X("w_out0"),
                  ln=X("ln0"), w_gate=X("wg0"), w_up=X("wu0"), w_down=X("wd0"), xout=x2scr, w_in=X("w_in1"),
                  b_in=X("b_in1"), qk_s=v4(qk1_s), v_s=v3(v1_s)))
    cc_chunks(P, qk1_s, qk1_g)
    cc_chunks(P, v1_s, v1_g)
    C.new_stage()
    stage_select(C, X("idxtab"), NIDX, [(qk1_g, qk1_r, 64 + NTT, 40), (v1_g, v1_r, 104 + NTT, NTT)])
    if cut == 4:
        return done()
    C.new_stage()
    stage_attn1(C, S, dict(qk_r=qk1_r, v_r=v1_r, o_s=o1_s, sinks=X("sinks"), ident=X("ident"), ms=X("ms")))
    cc_chunks(P, o1_s, o1_g)
    C.new_stage()
    stage_select(C, X("idxtab"), NIDX, [(o1_g, o1_r, 64, NTT)])
    if cut == 5:
        return done()
    C.new_stage()
    outs.append("xout")
    xout = C.dout("xout", [Tc, D], F32)
    TokStage(C, Tc, [("back", 1)],
             dict(common(), router_w=X("router_w"), router_b=X("router_b"), xin=x2scr, o_r=v3(o1_r), w_out=X("w_out1"),
                  ln=X("ln1"), w_gate=X("wg1"), w_up=X("wu1"), w_down=X("wd1"), xout=xout))
    return done()


_CUT = [6]


def kernel(x, c, positions, router_w, router_bias,
           l0_w_ada, l0_b_ada, l0_w_in, l0_lambda_q1, l0_lambda_k1, l0_lambda_q2, l0_lambda_k2,
           l0_subln_g, l0_w_out, l0_ln1_g, l0_ln1_b, l0_w_gate, l0_w_up, l0_w_down, l0_ln2_g, l0_ln2_b,
           l1_w_ada, l1_b_ada, l1_w_in, l1_b_in, l1_sinks, l1_w_out, l1_ln1_g, l1_ln1_b,
           l1_w_gate, l1_w_up, l1_w_down, l1_ln2_g, l1_ln2_b):
    f32 = lambda a: np.ascontiguousarray(np.asarray(a, dtype=np.float32))
    x = f32(x)
    B, S, _ = x.shape
    Tc = S // 4
    ident, r32t, invf = _consts()
    md, mmk, ms, E = _mask_consts(S)
    pos = np.ascontiguousarray(np.asarray(positions, dtype=np.int32))
    cores = [(b, g) for b in range(2) for g in range(4)]
    c32 = f32(c)
    wa = [f32(l0_w_ada).reshape(D, 6, 4, 1024), f32(l1_w_ada).reshape(D, 6, 4, 1024)]
    ba = [f32(l0_b_ada).reshape(6, 4, 1024), f32(l1_b_ada).reshape(6, 4, 1024)]
    wada = [[np.ascontiguousarray(wa[l][:, :, g, :]).reshape(D, 6144) for g in range(4)] for l in range(2)]
    bada = [[np.ascontiguousarray(ba[l][:, g, :]).reshape(1, 6144) for g in range(4)] for l in range(2)]
    sinks = f32(l1_sinks)
    shared = dict(
        ident=ident, r32t=r32t, invf=invf, md=md, mm=mmk, ms=ms, E=E,
        router_w=f32(router_w), router_b=f32(router_bias)[None, :],
        w_in0=f32(l0_w_in), lamv=np.stack([f32(l0_lambda_q1), f32(l0_lambda_k1), f32(l0_lambda_q2), f32(l0_lambda_k2)]),
        subg=f32(l0_subln_g)[None, :], w_out0=f32(l0_w_out),
        ln0=np.stack([f32(l0_ln1_g), f32(l0_ln1_b), f32(l0_ln2_g), f32(l0_ln2_b)]),
        wg0=f32(l0_w_gate), wu0=f32(l0_w_up), wd0=f32(l0_w_down),
        w_in1=f32(l1_w_in), b_in1=f32(l1_b_in)[None, :], w_out1=f32(l1_w_out),
        ln1=np.stack([f32(l1_ln1_g), f32(l1_ln1_b), f32(l1_ln2_g), f32(l1_ln2_b)]),
        wg1=f32(l1_w_gate), wu1=f32(l1_w_up), wd1=f32(l1_w_down))
    maps = []
    pp = np.arange(128)[:, None]
    for b, g in cores:
        nt_ = Tc // 128
        tabs = [_tab(g, 16, _r0(4 * 16 * 128, Tc)), _tab(g, nt_, _r0(4 * Tc, 1024)), _tab(g, 10, _r0(4 * 10 * 128, Tc)),
                _tab(g, nt_, _r0(4 * Tc, 256))]
        idxtab = np.ascontiguousarray(np.concatenate(tabs, axis=1).astype(np.int32))
        m = dict(shared, idxtab=idxtab)
        m.update(xin=np.ascontiguousarray(x[b, g * Tc:(g + 1) * Tc]), pos=np.ascontiguousarray(pos[b, g * Tc:(g + 1) * Tc][None, :]),
                 c_own=np.ascontiguousarray(c32[b][None, :]), wada0=wada[0][g], bada0=bada[0][g], wada1=wada[1][g], bada1=bada[1][g],
                 sinks=np.ascontiguousarray(sinks[None, 8 * g:8 * g + 8]))
        maps.append(m)
    nc, names, onames = build_all(S, _CUT[0], _CUT[0] < 6)
    maps = [{k: m[k] for k in names} for m in maps]
    res = run_bass_kernel_spmd(nc, maps, core_ids=list(range(8)))
    if _CUT[0] < 6:
        return [{k: np.asarray(res.results[i][k]) for k in onames} for i in range(8)]
    out = np.empty((B, S, D), np.float32)
    for i, (b, g) in enumerate(cores):
        out[b, g * Tc:(g + 1) * Tc] = res.results[i]["xout"]
    return out
```
